# Optimizing a Trainium2 kernel written in Bass

```python
import math
import jax, jax.numpy as jnp
from jax import lax
import numpy as np

D_MODEL = 4096
BATCH = 4
SEQ = 4096
DEPTH = 2

HEAD_DIM = 128
DIFF_HEADS = 8
DIFF_QK = 2 * HEAD_DIM
DIFF_V = 2 * HEAD_DIM
DIFF_QBLOCK = 128
ML_HEADS = 8
ML_QK = HEAD_DIM
ML_V = 2 * HEAD_DIM
ML_CHUNK = 64
CONV_W = 4
MOBA_HEADS = 16
MOBA_BLOCK = 256
MOBA_TOPK = 3
MOBA_QCHUNK = 16
N_BRANCH = 3
BRANCH_W = DIFF_HEADS * DIFF_V
D_FF = -(-8 * D_MODEL // (3 * 256)) * 256
EPS = 1e-6

IN_SIZES = (
    DIFF_HEADS * DIFF_QK, DIFF_HEADS * DIFF_QK, DIFF_HEADS * DIFF_V,
    ML_HEADS * ML_QK, ML_HEADS * ML_QK, ML_HEADS * ML_V, ML_HEADS * ML_V,
    ML_HEADS, ML_HEADS,
    MOBA_HEADS * HEAD_DIM, MOBA_HEADS * HEAD_DIM, MOBA_HEADS * HEAD_DIM,
    N_BRANCH * D_MODEL,
)
D_IN = int(sum(IN_SIZES))
SPLIT_AT = tuple(int(s) for s in np.cumsum(IN_SIZES)[:-1])

kernel_name = "hybrid_diffattn_mlstm_moba_swiglu"


def rms_norm(x, g):
    xf = x.astype(jnp.float32)
    y = xf * lax.rsqrt(jnp.mean(xf * xf, axis=-1, keepdims=True) + EPS)
    return (y * g.astype(jnp.float32)).astype(x.dtype)


def head_layer_norm(x, g):
    H, Dv = x.shape[-2], x.shape[-1]
    xf = x.astype(jnp.float32)
    mu = jnp.mean(xf, axis=-1, keepdims=True)
    xc = xf - mu
    y = xc * lax.rsqrt(jnp.mean(xc * xc, axis=-1, keepdims=True) + EPS)
    return y * g.astype(jnp.float32).reshape(H, Dv)


def alibi_slopes(n):
    return jnp.asarray(2.0 ** (-8.0 * np.arange(1, n + 1) / n), dtype=jnp.float32)


def causal_conv(x, w, b):
    C = x.shape[-1]
    y = lax.conv_general_dilated(
        x, w[:, None, :].astype(x.dtype), window_strides=(1,),
        padding=((w.shape[0] - 1, 0),), dimension_numbers=("NWC", "WIO", "NWC"),
        feature_group_count=C)
    return y + b.astype(x.dtype)


def diff_attention(q, k, v, lam, slopes):
    B, H, _, S, Dh = q.shape
    Dv = v.shape[-1]
    scale = Dh ** -0.5
    pos_k = jnp.arange(S)

    def block(i):
        t0 = i * DIFF_QBLOCK
        qb = lax.dynamic_slice_in_dim(q, t0, DIFF_QBLOCK, axis=3)
        pos_q = t0 + jnp.arange(DIFF_QBLOCK)
        s = jnp.einsum("bhcqd,bhckd->bhcqk", qb, k, preferred_element_type=jnp.float32) * scale
        dist = (pos_q[:, None] - pos_k[None, :]).astype(jnp.float32)
        s = jnp.where(dist >= 0, s - slopes[:, None, None, None] * dist, -jnp.inf)
        p = jax.nn.softmax(s, axis=-1)
        a = p[:, :, 0] - lam * p[:, :, 1]
        return jnp.einsum("bhqk,bhkd->bhqd", a.astype(v.dtype), v)

    out = lax.map(block, jnp.arange(S // DIFF_QBLOCK))
    return out.transpose(1, 0, 3, 2, 4).reshape(B, S, H, Dv)


def mlstm(q, k, v, i_pre, f_pre):
    B, S, H, Dk = q.shape
    Dv = v.shape[-1]
    L = ML_CHUNK
    NC = S // L
    f32 = jnp.float32

    def to_chunks(a):
        a = a.reshape((B, NC, L, H) + a.shape[3:])
        return jnp.moveaxis(a, (1, 3), (0, 2))

    qc = to_chunks(q.astype(f32))
    kc = to_chunks(k.astype(f32) * (Dk ** -0.5))
    vc = to_chunks(v.astype(f32))
    ic = to_chunks(i_pre.astype(f32))
    lfc = to_chunks(jax.nn.log_sigmoid(f_pre.astype(f32)))
    tri = jnp.tril(jnp.ones((L, L), dtype=bool))

    def step(carry, xs):
        C, n, m = carry
        qq, kk, vv, ii, lf = xs
        g = jnp.cumsum(lf, axis=-1)
        gL = g[..., -1]
        D = jnp.where(tri, g[..., :, None] - g[..., None, :] + ii[..., None, :], -jnp.inf)
        inter = g + m[..., None]
        m_t = jnp.maximum(inter, jnp.max(D, axis=-1))
        w = jnp.exp(D - m_t[..., None])
        a = jnp.exp(inter - m_t)
        sw = jnp.einsum("bhtd,bhsd->bhts", qq, kk) * w
        num = a[..., None] * jnp.einsum("bhvd,bhtd->bhtv", C, qq) + jnp.einsum("bhts,bhsv->bhtv", sw, vv)
        den = a * jnp.einsum("bhd,bhtd->bht", n, qq) + jnp.sum(sw, axis=-1)
        h = num / jnp.maximum(jnp.abs(den), jnp.exp(-m_t))[..., None]
        aL = gL[..., None] - g + ii
        m_new = jnp.maximum(gL + m, jnp.max(aL, axis=-1))
        wL = jnp.exp(aL - m_new[..., None])
        decay = jnp.exp(gL + m - m_new)
        C_new = decay[..., None, None] * C + jnp.einsum("bhs,bhsv,bhsd->bhvd", wL, vv, kk)
        n_new = decay[..., None] * n + jnp.einsum("bhs,bhsd->bhd", wL, kk)
        return (C_new, n_new, m_new), h

    init = (jnp.zeros((B, H, Dv, Dk), f32), jnp.zeros((B, H, Dk), f32), jnp.zeros((B, H), f32))
    _, hs = lax.scan(step, init, (qc, kc, vc, ic, lfc))
    return hs.transpose(1, 0, 3, 2, 4).reshape(B, S, H, Dv)


def moba_attention(q, k, v, slopes):
    B, S, H, Dh = q.shape
    BLK = MOBA_BLOCK
    QC = MOBA_QCHUNK
    NB = -(-S // BLK)
    Sp = NB * BLK
    f32 = jnp.float32
    scale = Dh ** -0.5
    q = q.transpose(0, 2, 1, 3)
    pad = ((0, 0), (0, 0), (0, Sp - S), (0, 0))
    kp = jnp.pad(k.transpose(0, 2, 1, 3), pad)
    vp = jnp.pad(v.transpose(0, 2, 1, 3), pad)
    kb = kp.reshape(B, H, NB, BLK, Dh)
    vb = vp.reshape(B, H, NB, BLK, Dh)
    kmean = jnp.mean(kb.astype(f32), axis=3)
    n_sel = min(MOBA_TOPK, NB)
    bi = jnp.arange(B)[:, None, None, None]
    hi = jnp.arange(H)[None, :, None, None]
    blk_ids = jnp.arange(NB)
    off = jnp.arange(BLK)

    def chunk(c):
        t0 = c * QC
        qc = lax.dynamic_slice_in_dim(q, t0, QC, axis=2)
        pos_q = t0 + jnp.arange(QC)
        own = t0 // BLK
        gs = jnp.einsum("bhqd,bhnd->bhqn", qc.astype(f32), kmean)
        gs = jnp.where(blk_ids < own, gs, -jnp.inf)
        _, idx = lax.top_k(gs, n_sel)
        valid = idx < own
        ks = kb[bi, hi, idx]
        vs = vb[bi, hi, idx]
        s_sel = jnp.einsum("bhqd,bhqnkd->bhqnk", qc, ks, preferred_element_type=f32) * scale
        dist_sel = (pos_q[:, None, None] - (idx[..., None] * BLK + off)).astype(f32)
        s_sel = jnp.where(valid[..., None], s_sel - slopes[:, None, None, None] * dist_sel, -jnp.inf)
        k_own = lax.dynamic_slice_in_dim(kp, own * BLK, BLK, axis=2)
        v_own = lax.dynamic_slice_in_dim(vp, own * BLK, BLK, axis=2)
        s_own = jnp.einsum("bhqd,bhkd->bhqk", qc, k_own, preferred_element_type=f32) * scale
        dist_own = (pos_q[:, None] - (own * BLK + off)[None, :]).astype(f32)
        s_own = jnp.where(dist_own >= 0, s_own - slopes[:, None, None] * dist_own, -jnp.inf)
        s = jnp.concatenate([s_sel.reshape(B, H, QC, n_sel * BLK), s_own], axis=-1)
        p = jax.nn.softmax(s, axis=-1).astype(v.dtype)
        p_sel = p[..., : n_sel * BLK].reshape(B, H, QC, n_sel, BLK)
        p_own = p[..., n_sel * BLK:]
        return (jnp.einsum("bhqnk,bhqnkd->bhqd", p_sel, vs)
                + jnp.einsum("bhqk,bhkd->bhqd", p_own, v_own))

    out = lax.map(chunk, jnp.arange(S // QC))
    return out.transpose(1, 0, 3, 2, 4).reshape(B, S, H * Dh)


def mixer_sublayer(x, layer_idx, norm1_g, w_in, diff_lambda, diff_norm_g, ml_conv_w, ml_conv_b,
                   ml_gate_b, ml_norm_g, w_branch, w_out):
    B, S, _ = x.shape
    h = rms_norm(x, norm1_g)
    z = h @ w_in
    (dq, dk, dv, mq, mk, mv, mo, mi, mf, bq, bk, bv, gates) = jnp.split(z, SPLIT_AT, axis=-1)

    lam_init = 0.8 - 0.6 * math.exp(-0.3 * layer_idx)
    lam_p = diff_lambda.astype(jnp.float32)
    lam = jnp.exp(jnp.sum(lam_p[0] * lam_p[1])) - jnp.exp(jnp.sum(lam_p[2] * lam_p[3])) + lam_init
    qa = dq.reshape(B, S, DIFF_HEADS, 2, HEAD_DIM).transpose(0, 2, 3, 1, 4)
    ka = dk.reshape(B, S, DIFF_HEADS, 2, HEAD_DIM).transpose(0, 2, 3, 1, 4)
    va = dv.reshape(B, S, DIFF_HEADS, DIFF_V).transpose(0, 2, 1, 3)
    ya = diff_attention(qa, ka, va, lam, alibi_slopes(DIFF_HEADS))
    ya = (rms_norm(ya, diff_norm_g) * (1.0 - lam_init)).reshape(B, S, BRANCH_W)

    qk = jax.nn.silu(causal_conv(jnp.concatenate([mq, mk], axis=-1), ml_conv_w, ml_conv_b))
    q_m, k_m = jnp.split(qk, 2, axis=-1)
    hm = mlstm(q_m.reshape(B, S, ML_HEADS, ML_QK), k_m.reshape(B, S, ML_HEADS, ML_QK),
               mv.reshape(B, S, ML_HEADS, ML_V), mi + ml_gate_b[0], mf + ml_gate_b[1])
    yb = head_layer_norm(hm, ml_norm_g).reshape(B, S, BRANCH_W) * jax.nn.sigmoid(mo.astype(jnp.float32))
    yb = yb.astype(x.dtype)

    yc = moba_attention(bq.reshape(B, S, MOBA_HEADS, HEAD_DIM), bk.reshape(B, S, MOBA_HEADS, HEAD_DIM),
                        bv.reshape(B, S, MOBA_HEADS, HEAD_DIM), alibi_slopes(MOBA_HEADS))

    g = jax.nn.sigmoid(gates.astype(jnp.float32)).reshape(B, S, N_BRANCH, D_MODEL).astype(x.dtype)
    merged = g[:, :, 0] * (ya @ w_branch[0])
    merged = merged + g[:, :, 1] * (yb @ w_branch[1])
    merged = merged + g[:, :, 2] * (yc @ w_branch[2])
    return merged @ w_out


def swiglu(x, w_gate_up, w_down):
    gu = x @ w_gate_up
    gate, up = jnp.split(gu, 2, axis=-1)
    return (jax.nn.silu(gate) * up) @ w_down


def setup_inputs(seed: int = 0) -> dict:
    key = jax.random.key(seed)
    ks = jax.random.split(key, 16)
    f32 = jnp.float32
    nrm = lambda k, shape, s: jax.random.normal(k, shape, f32) * s
    gate_b = jnp.stack([
        nrm(ks[6], (DEPTH, ML_HEADS), 0.1),
        3.0 + 3.0 * jax.random.uniform(ks[7], (DEPTH, ML_HEADS), f32),
    ], axis=1)
    return {
        "x": nrm(ks[0], (BATCH, SEQ, D_MODEL), 1.0),
        "norm1_g": 1.0 + nrm(ks[1], (DEPTH, D_MODEL), 0.02),
        "w_in": nrm(ks[2], (DEPTH, D_MODEL, D_IN), D_MODEL ** -0.5),
        "diff_lambda": nrm(ks[3], (DEPTH, 4, HEAD_DIM), 0.1),
        "diff_norm_g": 1.0 + nrm(ks[4], (DEPTH, DIFF_V), 0.02),
        "ml_conv_w": nrm(ks[5], (DEPTH, CONV_W, 2 * ML_HEADS * ML_QK), CONV_W ** -0.5),
        "ml_conv_b": nrm(ks[8], (DEPTH, 2 * ML_HEADS * ML_QK), 0.01),
        "ml_gate_b": gate_b,
        "ml_norm_g": 1.0 + nrm(ks[9], (DEPTH, ML_HEADS * ML_V), 0.02),
        "w_branch": nrm(ks[10], (DEPTH, N_BRANCH, BRANCH_W, D_MODEL), BRANCH_W ** -0.5),
        "w_out": nrm(ks[11], (DEPTH, D_MODEL, D_MODEL), D_MODEL ** -0.5),
        "norm2_g": 1.0 + nrm(ks[12], (DEPTH, D_MODEL), 0.02),
        "w_gate_up": nrm(ks[13], (DEPTH, D_MODEL, 2 * D_FF), D_MODEL ** -0.5),
        "w_down": nrm(ks[14], (DEPTH, D_FF, D_MODEL), D_FF ** -0.5),
        "final_g": 1.0 + nrm(ks[15], (D_MODEL,), 0.02),
    }


def reference(x, norm1_g, w_in, diff_lambda, diff_norm_g, ml_conv_w, ml_conv_b, ml_gate_b, ml_norm_g,
              w_branch, w_out, norm2_g, w_gate_up, w_down, final_g):
    for l in range(DEPTH):
        x = x + mixer_sublayer(x, l, norm1_g[l], w_in[l], diff_lambda[l], diff_norm_g[l], ml_conv_w[l],
                               ml_conv_b[l], ml_gate_b[l], ml_norm_g[l], w_branch[l], w_out[l]).astype(x.dtype)
        x = x + swiglu(rms_norm(x, norm2_g[l]), w_gate_up[l], w_down[l]).astype(x.dtype)
    return rms_norm(x, final_g)
```

```python
import math
from contextlib import ExitStack
import numpy as np
import concourse.bass as bass
import concourse.mybir as mybir
from concourse.bass_utils import run_bass_kernel_spmd

F32 = mybir.dt.float32
BF16 = mybir.dt.bfloat16
AF = mybir.ActivationFunctionType
ALU = mybir.AluOpType
EPS = 1e-6
SAME_ENG_SYNC = True


_UID = [0]


class Buf:
    def __init__(self, name, loose=False):
        _UID[0] += 1
        self.uid = _UID[0]
        self.name = name
        self.w = {}
        self.r = {}
        self.sem = None
        self.semv = 0
        self.loose = loose


class Ins:
    __slots__ = ("eng", "fn", "waits", "sig", "cnt", "idx", "dbuf")

    def __init__(self, eng, fn, idx):
        self.eng, self.fn, self.idx = eng, fn, idx
        self.waits = []
        self.sig = False
        self.cnt = 0
        self.dbuf = None


ENGS = ("pe", "act", "dve", "pool", "sp")


class Prog:
    def __init__(self, nc, stack):
        self.nc = nc
        self.stack = stack
        self.q = {e: [] for e in ENGS}
        self.known = {e: {} for e in ENGS}
        self.dbufs = []
        self.esem = {}
        self.nidx = {e: 0 for e in ENGS}
        self.ccnt = {e: 0 for e in ENGS}
        self.last = {e: None for e in ENGS}
        self.sempool = []
        self.nsem = 0
        for e in ENGS:
            self.esem[e] = self.stack.enter_context(nc.semaphore("e_" + e))

    def _new(self, eng, fn):
        ins = Ins(eng, fn, self.nidx[eng])
        self.nidx[eng] += 1
        self.q[eng].append(ins)
        return ins

    def _deps(self, eng, reads, writes, is_dma):
        deps = {}

        def add(ev):
            if ev[0] == "E":
                key, order = ev[1].eng, ev[1].idx
            else:
                key, order = ev[1].uid, ev[2]
            if key not in deps or deps[key][1] < order:
                deps[key] = (ev, order)

        for b in reads:
            for ev in b.w.values():
                add(ev)
        for b in writes:
            if b.loose:
                continue
            for ev in b.w.values():
                add(ev)
            for ev in b.r.values():
                add(ev)
        out = []
        for key, (ev, order) in deps.items():
            if ev[0] == "E" and ev[1].eng == eng:
                if eng in ("pe", "sp") or is_dma or not SAME_ENG_SYNC:
                    continue
            if self.known[eng].get(key, -1) >= order:
                continue
            self.known[eng][key] = order
            out.append(ev)
        return out

    def op(self, eng, fn, reads=(), writes=()):
        ins = self._new(eng, fn)
        self.last[eng] = ins
        ins.waits = self._deps(eng, reads, writes, False)
        ev = ("E", ins)
        for b in reads:
            b.r[eng] = ev
        for b in writes:
            b.w = {eng: ev}
            b.r = {}
        return ins

    def dma(self, eng, out, in_, sb, reads=(), writes=(), slow=False):
        if sb.sem is None:
            if self.sempool:
                sb.sem, sb.semv = self.sempool.pop()
            else:
                self.nsem += 1
                sb.sem, sb.semv = self.stack.enter_context(self.nc.semaphore(f"d_{self.nsem}")), 0
            self.dbufs.append(sb)
        if slow:
            fn = lambda e: e.dma_start(out=out, in_=in_, allow_slow_non_contiguous=True)
        else:
            fn = lambda e: e.dma_start(out=out, in_=in_)
        ins = self._new(eng, fn)
        ins.waits = self._deps(eng, reads, writes, True)
        ins.dbuf = sb
        sb.semv += 16
        ev = ("D", sb, sb.semv)
        key = sb.uid
        for b in reads:
            if not b.loose:
                b.r[key] = ev
        for b in writes:
            if b.loose:
                b.w[key] = ev
            else:
                b.w = {key: ev}
                b.r = {}
        return ins

    def end_phase(self):
        last = dict(self.last)
        dsnap = [(b, b.semv) for b in self.dbufs]
        for e in ENGS:
            ins = self._new(e, None)
            for e2 in ENGS:
                if e2 != e and last[e2] is not None:
                    if self.known[e].get(e2, -1) < last[e2].idx:
                        self.known[e][e2] = last[e2].idx
                        ins.waits.append(("E", last[e2]))
            for b, v in dsnap:
                if self.known[e].get(b.uid, -1) < v:
                    self.known[e][b.uid] = v
                    ins.waits.append(("D", b, v))
        self.flush()
        for b in self.dbufs:
            self.sempool.append((b.sem, b.semv))
            b.sem = None
        self.dbufs = []

    def flush(self):
        nc = self.nc
        for e in ENGS:
            for ins in self.q[e]:
                for ev in ins.waits:
                    if ev[0] == "E":
                        ev[1].sig = True
        for e in ENGS:
            c = self.ccnt[e]
            for ins in self.q[e]:
                if ins.sig:
                    assert ins.fn is not None
                    c += 1
                ins.cnt = c
            self.ccnt[e] = c
        esem = self.esem
        qs = self.q
        self.q = {e: [] for e in ENGS}

        def run(e, eng):
            for ins in qs[e]:
                for ev in ins.waits:
                    if ev[0] == "E":
                        eng.wait_ge(esem[ev[1].eng], ev[1].cnt)
                    else:
                        eng.wait_ge(ev[1].sem, ev[2])
                if ins.fn is None:
                    continue
                r = ins.fn(eng)
                if ins.dbuf is not None:
                    r.then_inc(ins.dbuf.sem, 16)
                elif ins.sig:
                    r.then_inc(esem[e], 1)

        with nc.Block() as block:
            @block.tensor
            def _(eng):
                run("pe", eng)

            @block.scalar
            def _(eng):
                run("act", eng)

            @block.vector
            def _(eng):
                run("dve", eng)

            @block.gpsimd
            def _(eng):
                run("pool", eng)

            @block.sync
            def _(eng):
                run("sp", eng)


class Tile:
    def __init__(self, t, name):
        self.t = t
        self.b = Buf(name)

    def __getitem__(self, k):
        return self.t[k]


class Ctx:
    def __init__(self, nc, p, stack):
        self.nc, self.p, self.stack = nc, p, stack
        self.n = 0

    def sb(self, shape, dt, name, stack=None):
        self.n += 1
        t = (stack or self.stack).enter_context(self.nc.sbuf_tensor(f"{name}_{self.n}", list(shape), dt))
        return Tile(t, f"{name}_{self.n}")

    def ps(self, shape, dt, name, stack=None):
        self.n += 1
        t = (stack or self.stack).enter_context(self.nc.psum_tensor(f"{name}_{self.n}", list(shape), dt))
        return Tile(t, f"{name}_{self.n}")


class Ring:
    def __init__(self, tiles):
        self.tiles = tiles
        self.i = 0

    def next(self):
        t = self.tiles[self.i % len(self.tiles)]
        self.i += 1
        return t


def cfg_full():
    return dict(D=4096, S=4096, L=2, NHD=8, NHM=8, NHB=16, DFF=11008, BLK=256, TOPK=3)


def col_offsets(c):
    D, NHD, NHM, NHB = c["D"], c["NHD"], c["NHM"], c["NHB"]
    sizes = [NHD * 256, NHD * 256, NHD * 256, NHM * 128, NHM * 128, NHM * 256, NHM * 256, NHM, NHM,
             NHB * 128, NHB * 128, NHB * 128, 3 * D]
    names = ["dq", "dk", "dv", "mq", "mk", "mv", "mo", "mi", "mf", "bq", "bk", "bv", "gates"]
    off = {}
    o = 0
    for n, s in zip(names, sizes):
        off[n] = o
        o += s
    off["_total"] = o
    return off


def build(c):
    D, S, L, NHD, NHM, NHB, DFF = c["D"], c["S"], c["L"], c["NHD"], c["NHM"], c["NHB"], c["DFF"]
    BLK, TOPK = c["BLK"], c["TOPK"]
    KC = D // 128
    BW = NHD * 256
    assert BW == NHM * 256 == NHB * 128
    OFF = col_offsets(c)
    DIN = OFF["_total"]
    NT = S // 128
    NB = S // BLK
    SW = S + 384

    nc = bass.Bass("TRN2", target_bir_lowering=False)

    def din(name, shape):
        return nc.dram_tensor(name, list(shape), F32, kind="ExternalInput").ap()

    xT_in = din("xT", [D, S])
    norm1_g = din("norm1_g", [L, D])
    w_in = din("w_in", [L, D, DIN])
    diff_lambda = din("diff_lambda", [L, 4, 128])
    diff_norm_g = din("diff_norm_g", [L, 256])
    ml_conv_w = din("ml_conv_w", [L, 4, 2 * NHM * 128])
    ml_conv_b = din("ml_conv_b", [L, 2 * NHM * 128])
    ml_gate_b = din("ml_gate_b", [L, 2, NHM])
    ml_norm_g = din("ml_norm_g", [L, NHM * 256])
    w_branch = din("w_branch", [L, 3, BW, D])
    w_out = din("w_out", [L, D, D])
    norm2_g = din("norm2_g", [L, D])
    w_gate_up = din("w_gate_up", [L, D, 2 * DFF])
    w_down = din("w_down", [L, DFF, D])
    final_g = din("final_g", [1, D])
    yT_out = nc.dram_tensor("yT", [D, S], F32, kind="ExternalOutput").ap()

    def scratch(name, shape, dt):
        return nc.dram_tensor(name, list(shape), dt, kind="Internal").ap()

    NQK = 2 * NHD * 256 + 2 * NHM * 128 + NHM * 256 + 2 * NHB * 128
    NV = NHD * 256 + NHM * 256 + NHB * 128
    scr = []
    for l in range(L):
        scr.append(dict(
            hT=scratch(f"hT{l}", [D, S], BF16), qk=scratch(f"qk{l}", [NQK, S], BF16),
            v=scratch(f"v{l}", [S, NV], BF16), gif=scratch(f"gif{l}", [S, 2 * NHM], F32),
            gates=scratch(f"gates{l}", [3 * D, S], BF16), y=scratch(f"y{l}", [3 * BW, S], BF16),
            merged=scratch(f"merged{l}", [D, S], BF16), x1=scratch(f"x1_{l}", [D, S], F32),
            h2=scratch(f"h2_{l}", [D, S], BF16), act=scratch(f"act{l}", [DFF, S], BF16),
            x2=scratch(f"x2_{l}", [D, S], F32), xh=scratch(f"xh{l}", [D, S], F32)))
    QO = {}
    o = 0
    for n, s in (("dq", NHD * 256), ("dk", NHD * 256), ("mq", NHM * 128), ("mk", NHM * 128), ("mo", NHM * 256),
                 ("bq", NHB * 128), ("bk", NHB * 128)):
        QO[n] = o
        o += s
    VO = {"dv": 0, "mv": NHD * 256, "bv": NHD * 256 + NHM * 256}

    top = ExitStack()
    p = Prog(nc, top)
    cx = Ctx(nc, p, top)
    dram = {}

    def dbuf(ap_name):
        if ap_name not in dram:
            dram[ap_name] = Buf(ap_name, loose=True)
        return dram[ap_name]

    ones_bf = cx.sb([128, 128], BF16, "ones_bf")
    ones_f = cx.sb([128, 128], F32, "ones_f")
    ident_f = cx.sb([128, 128], F32, "ident_f")
    utri_f = cx.sb([128, 128], F32, "utri_f")
    iot = cx.sb([128, 128], F32, "iot")
    p.op("pool", lambda e: e.memset(ones_bf[:], 1.0), writes=[ones_bf.b])
    p.op("pool", lambda e: e.memset(ones_f[:], 1.0), writes=[ones_f.b])
    eps_t = cx.sb([128, 2], F32, "eps_t")
    p.op("pool", lambda e: e.memset(eps_t[:, 0:1], EPS), writes=[eps_t.b])
    p.op("pool", lambda e: e.memset(eps_t[:, 1:2], 1.0), writes=[eps_t.b])
    p.op("pool", lambda e: e.iota(iot[:], pattern=[[1, 128]], base=0, channel_multiplier=-1,
                                  allow_small_or_imprecise_dtypes=True), writes=[iot.b])
    p.op("dve", lambda e: e.tensor_single_scalar(out=ident_f[:], in_=iot[:], scalar=0.0, op=ALU.is_equal),
         reads=[iot.b], writes=[ident_f.b])
    p.op("dve", lambda e: e.tensor_single_scalar(out=utri_f[:], in_=iot[:], scalar=0.0, op=ALU.is_ge),
         reads=[iot.b], writes=[utri_f.b])
    p.end_phase()

    def phase_rmsnorm(src, src_name, g_ap, dst, dst_name, out_f32=False):
        TT = 256
        with ExitStack() as st:
            gt = cx.sb([128, KC], F32, "nrm_g", st)
            p.dma("sp", gt[:], g_ap.rearrange("o (c p) -> p (o c)", p=128), gt.b, writes=[gt.b], slow=True)
            X = [cx.sb([128, KC, TT], F32, "nrm_x", st) for _ in range(2)]
            SQ = cx.sb([128, KC, TT], BF16, "nrm_sq", st)
            H = [cx.sb([128, KC, TT], F32 if out_f32 else BF16, "nrm_h", st) for _ in range(2)]
            R = cx.sb([128, TT], F32, "nrm_r", st)
            PS = cx.ps([128, TT], F32, "nrm_ps", st)
            sview = src.rearrange("(c p) t -> p c t", p=128)
            dview = dst.rearrange("(c p) t -> p c t", p=128)
            n = S // TT
            for i in range(n):
                x = X[i % 2]
                h = H[i % 2]
                for k0 in range(0, KC, 8):
                    k1 = min(KC, k0 + 8)
                    p.dma("sp", x[:, k0:k1, :], sview[:, k0:k1, i * TT:(i + 1) * TT], x.b, reads=[dbuf(src_name)], writes=[x.b])
                p.op("act", lambda e, x=x: e.activation(out=SQ[:], in_=x[:], func=AF.Square), reads=[x.b], writes=[SQ.b])
                for k in range(KC):
                    p.op("pe", lambda e, k=k: e.matmul(PS[:], ones_bf[:], SQ[:, k, :], start=(k == 0), stop=(k == KC - 1)),
                         reads=[SQ.b, ones_bf.b], writes=[PS.b])
                p.op("act", lambda e: e.activation(out=R[:], in_=PS[:], func=AF.Sqrt, bias=eps_t[:, 0:1], scale=1.0 / D),
                     reads=[PS.b, eps_t.b], writes=[R.b])
                p.op("dve", lambda e: e.reciprocal(out=R[:], in_=R[:]), reads=[R.b], writes=[R.b])
                for k in range(KC):
                    p.op("dve", lambda e, k=k, x=x, h=h: e.scalar_tensor_tensor(out=h[:, k, :], in0=x[:, k, :], scalar=gt[:, k:k + 1],
                                                                               in1=R[:], op0=ALU.mult, op1=ALU.mult),
                         reads=[x.b, R.b, gt.b], writes=[h.b])
                for k0 in range(0, KC, 8):
                    k1 = min(KC, k0 + 8)
                    p.dma("pool", dview[:, k0:k1, i * TT:(i + 1) * TT], h[:, k0:k1, :], h.b, reads=[h.b], writes=[dbuf(dst_name)])
            p.end_phase()

    def phase_gemm(name, acts, K, TP, NG, jobs_for_group, ncolgroups, tokmajor=False, nws=4):
        KCk = (K + 127) // 128
        assert K % 128 == 0
        PIECE = 8
        with ExitStack() as st:
            A = [cx.sb([128, KCk, TP], BF16, f"{name}_a{i}", st) for i in range(len(acts))]
            nacc = len(jobs_for_group(0)[0])
            WST = Ring([cx.sb([128, PIECE, NG], F32, f"{name}_ws", st) for _ in range(nws)])
            WSL = [Ring([cx.sb([128, KCk, NG], BF16, f"{name}_wb{a}", st) for _ in range(2)]) for a in range(nacc)]
            NTT = TP // 512 if not tokmajor else TP // 128
            banks = Ring([cx.ps([128, 512], F32, f"{name}_ps", st) for _ in range(8)])
            cast_i = [0]
            for tp0 in range(0, S, TP):
                for i, (a_ap, a_name) in enumerate(acts):
                    av = a_ap.rearrange("(c p) t -> p c t", p=128)
                    for k0 in range(0, KCk, 16):
                        k1 = min(KCk, k0 + 16)
                        p.dma("sp", A[i][:, k0:k1, :], av[:, k0:k1, tp0:tp0 + TP], A[i].b, reads=[dbuf(a_name)], writes=[A[i].b])
                for g in range(ncolgroups):
                    wspecs, epi = jobs_for_group(g)
                    slabs = []
                    for a, (w2d, col0, ai) in enumerate(wspecs):
                        slab = WSL[a].next()
                        wv = w2d.rearrange("(c p) n -> p c n", p=128)
                        for k0 in range(0, KCk, PIECE):
                            k1 = min(KCk, k0 + PIECE)
                            ws = WST.next()
                            p.dma("sp", ws[:, 0:k1 - k0, :], wv[:, k0:k1, col0:col0 + NG], ws.b, writes=[ws.b])
                            eng = ("dve", "act")[cast_i[0] % 2]
                            cast_i[0] += 1
                            if eng == "dve":
                                p.op("dve", lambda e, ws=ws, slab=slab, k0=k0, k1=k1: e.tensor_copy(out=slab[:, k0:k1, :], in_=ws[:, 0:k1 - k0, :]),
                                     reads=[ws.b], writes=[slab.b])
                            else:
                                p.op("act", lambda e, ws=ws, slab=slab, k0=k0, k1=k1: e.copy(out=slab[:, k0:k1, :], in_=ws[:, 0:k1 - k0, :]),
                                     reads=[ws.b], writes=[slab.b])
                        slabs.append(slab)
                    if not tokmajor:
                        for ct in range(NG // 128):
                            accs = []
                            for a, (w2d, col0, ai) in enumerate(wspecs):
                                tiles = [banks.next() for _ in range(NTT)]
                                for k in range(KCk):
                                    for tt in range(NTT):
                                        p.op("pe", lambda e, a=a, k=k, tt=tt, ct=ct, ai=ai, tl=tiles[tt], sl=slabs[a]:
                                             e.matmul(tl[:], sl[:, k, ct * 128:(ct + 1) * 128], A[ai][:, k, tt * 512:(tt + 1) * 512],
                                                      start=(k == 0), stop=(k == KCk - 1)),
                                             reads=[slabs[a].b, A[ai].b], writes=[tiles[tt].b])
                                accs.append(tiles)
                            epi(tp0, g, ct, accs)
                    else:
                        for tt in range(NTT):
                            tl = banks.next()
                            for k in range(KCk):
                                p.op("pe", lambda e, k=k, tt=tt, tl=tl, sl=slabs[0]:
                                     e.matmul(tl[:, 0:NG], A[0][:, k, tt * 128:(tt + 1) * 128], sl[:, k, :],
                                              start=(k == 0), stop=(k == KCk - 1)),
                                     reads=[slabs[0].b, A[0].b], writes=[tl.b])
                            epi(tp0, g, tt, tl)
            p.end_phase()

    for l in range(L):
        sc = scr[l]
        x_src, x_src_name = (xT_in, "xT_in") if l == 0 else (scr[l - 1]["x2"], f"x2_{l - 1}")
        nm = lambda s: f"{s}{l}"
        CUT = c.get("cut", 99)
        phase_rmsnorm(x_src, x_src_name, norm1_g[l:l + 1, :], sc["hT"], nm("hT"))
        if CUT <= 1:
            break

        with ExitStack() as st:
            NG = 256 if (NHM * 128) % 256 == 0 else 128
            OST = Ring([cx.sb([128, 512], BF16, "zin_o", st) for _ in range(4)])
            groups = []
            for n_, sz in (("dq", NHD * 256), ("dk", NHD * 256), ("mq", NHM * 128), ("mk", NHM * 128), ("mo", NHM * 256),
                           ("bq", NHB * 128), ("bk", NHB * 128)):
                for c0 in range(0, sz, NG):
                    groups.append((OFF[n_] + c0, "qk", QO[n_] + c0, n_ == "mo"))
            for c0 in range(0, 3 * D, NG):
                groups.append((OFF["gates"] + c0, "gates", c0, True))

            def jobs(g):
                col0, dst, row0, sig = groups[g]

                def epi(tp0, g_, ct, accs, dst=dst, row0=row0, sig=sig):
                    for tt, tl in enumerate(accs[0]):
                        o = OST.next()
                        if sig:
                            p.op("act", lambda e, o=o, tl=tl: e.activation(out=o[:], in_=tl[:], func=AF.Sigmoid),
                                 reads=[tl.b], writes=[o.b])
                        else:
                            p.op("dve", lambda e, o=o, tl=tl: e.tensor_copy(out=o[:], in_=tl[:]), reads=[tl.b], writes=[o.b])
                        r0 = row0 + ct * 128
                        t0 = tp0 + tt * 512
                        p.dma("pool", sc[dst][r0:r0 + 128, t0:t0 + 512], o[:], o.b, reads=[o.b], writes=[dbuf(nm(dst))])
                return [(w_in[l], col0, 0)], epi
            TP = min(c.get("TPBIG", 2048), S)
            phase_gemm("zin", [(sc["hT"], nm("hT"))], D, TP, NG, jobs, len(groups), nws=2 if TP > 1024 else 4)
        if CUT <= 2:
            break

        with ExitStack() as st:
            NG = 256
            OST = Ring([cx.sb([128, NG], BF16, "zv_o", st) for _ in range(4)])
            OSF = Ring([cx.sb([128, 2 * NHM], F32, "zv_of", st) for _ in range(2)])
            groups = []
            for n_, sz in (("dv", NHD * 256), ("mv", NHM * 256), ("bv", NHB * 128)):
                for c0 in range(0, sz, NG):
                    groups.append((OFF[n_] + c0, VO[n_] + c0, NG))
            groups.append((OFF["mi"], -1, 2 * NHM))

            def jobs(g):
                col0, vcol, width = groups[g]

                def epi(tp0, g_, tt, tl, vcol=vcol, width=width):
                    t0 = tp0 + tt * 128
                    if vcol >= 0:
                        o = OST.next()
                        p.op("dve", lambda e, o=o, tl=tl: e.tensor_copy(out=o[:], in_=tl[:, 0:NG]), reads=[tl.b], writes=[o.b])
                        p.dma("pool", sc["v"][t0:t0 + 128, vcol:vcol + NG], o[:], o.b, reads=[o.b], writes=[dbuf(nm("v"))])
                    else:
                        o = OSF.next()
                        p.op("dve", lambda e, o=o, tl=tl: e.tensor_copy(out=o[:], in_=tl[:, 0:width]), reads=[tl.b], writes=[o.b])
                        p.dma("pool", sc["gif"][t0:t0 + 128, :], o[:], o.b, reads=[o.b], writes=[dbuf(nm("gif"))])
                return [(w_in[l], col0, 0)], epi
            TPv = min(c.get("TPBIG", 2048), S)
            phase_gemm("zv", [(sc["hT"], nm("hT"))], D, TPv, NG, jobs, len(groups), tokmajor=True, nws=2 if TPv > 1024 else 4)
        if CUT <= 3:
            break

        mixers(nc, p, cx, c, l, sc, nm, dbuf, dict(ones_bf=ones_bf, ones_f=ones_f, ident_f=ident_f, utri_f=utri_f, eps_t=eps_t),
               dict(diff_lambda=diff_lambda, diff_norm_g=diff_norm_g, ml_conv_w=ml_conv_w, ml_conv_b=ml_conv_b,
                    ml_gate_b=ml_gate_b, ml_norm_g=ml_norm_g), QO, VO)
        if CUT <= 6:
            break

        with ExitStack() as st:
            NG = 128
            GT = Ring([cx.sb([128, 512], BF16, "br_g", st) for _ in range(6)])
            MT = Ring([cx.sb([128, 512], F32, "br_m", st) for _ in range(2)])
            OT = Ring([cx.sb([128, 512], BF16, "br_o", st) for _ in range(2)])

            def jobs(g):
                col0 = g * NG

                def epi(tp0, g_, ct, accs, col0=col0):
                    r0 = col0 + ct * 128
                    for tt in range(len(accs[0])):
                        t0 = tp0 + tt * 512
                        gts = []
                        for b in range(3):
                            gt_ = GT.next()
                            p.dma("sp", gt_[:], sc["gates"][b * D + r0:b * D + r0 + 128, t0:t0 + 512], gt_.b,
                                  reads=[dbuf(nm("gates"))], writes=[gt_.b])
                            gts.append(gt_)
                        m = MT.next()
                        o = OT.next()
                        p.op("dve", lambda e, m=m, a=accs[0][tt], g0=gts[0]: e.tensor_tensor(out=m[:], in0=a[:], in1=g0[:], op=ALU.mult),
                             reads=[accs[0][tt].b, gts[0].b], writes=[m.b])
                        for b in (1, 2):
                            gt_ = gts[b]
                            p.op("dve", lambda e, a=accs[b][tt], gt_=gt_: e.tensor_tensor(out=gt_[:], in0=a[:], in1=gt_[:], op=ALU.mult),
                                 reads=[accs[b][tt].b, gt_.b], writes=[gt_.b])
                            p.op("pool", lambda e, m=m, gt_=gt_: e.tensor_tensor(out=m[:], in0=m[:], in1=gt_[:], op=ALU.add),
                                 reads=[m.b, gt_.b], writes=[m.b])
                        p.op("act", lambda e, o=o, m=m: e.copy(out=o[:], in_=m[:]), reads=[m.b], writes=[o.b])
                        p.dma("pool", sc["merged"][r0:r0 + 128, t0:t0 + 512], o[:], o.b, reads=[o.b], writes=[dbuf(nm("merged"))])
                return [(w_branch[l, b], col0, b) for b in range(3)], epi
            acts = [(sc["y"][b * BW:(b + 1) * BW, :], nm("y")) for b in range(3)]
            phase_gemm("br", acts, BW, min(1024, S), NG, jobs, D // NG)
        if CUT <= 7:
            break

        def resid_gemm(name, act_ap, act_name, K, TP, NG, w2d, xs, xs_name, xd, xd_name):
            with ExitStack() as st:
                XT = Ring([cx.sb([128, 512], F32, name + "_x", st) for _ in range(3)])

                def jobs(g):
                    col0 = g * NG

                    def epi(tp0, g_, ct, accs, col0=col0):
                        r0 = col0 + ct * 128
                        for tt, tl in enumerate(accs[0]):
                            t0 = tp0 + tt * 512
                            xt = XT.next()
                            p.dma("sp", xt[:], xs[r0:r0 + 128, t0:t0 + 512], xt.b, reads=[dbuf(xs_name)], writes=[xt.b])
                            p.op("dve", lambda e, xt=xt, tl=tl: e.tensor_tensor(out=xt[:], in0=tl[:], in1=xt[:], op=ALU.add),
                                 reads=[tl.b, xt.b], writes=[xt.b])
                            p.dma("pool", xd[r0:r0 + 128, t0:t0 + 512], xt[:], xt.b, reads=[xt.b], writes=[dbuf(xd_name)])
                    return [(w2d, col0, 0)], epi
                phase_gemm(name, [(act_ap, act_name)], K, TP, NG, jobs, D // NG, nws=2 if TP > 1024 else 4)

        resid_gemm("wo", sc["merged"], nm("merged"), D, min(c.get("TPBIG", 2048), S), 256, w_out[l], x_src, x_src_name, sc["x1"], nm("x1_"))

        phase_rmsnorm(sc["x1"], nm("x1_"), norm2_g[l:l + 1, :], sc["h2"], nm("h2_"))
        with ExitStack() as st:
            TPg = min(c.get("TPBIG", 2048), S)
            NG = 256 if (DFF % 256 == 0 and TPg <= 1024) else 128
            ST = Ring([cx.sb([128, 512], F32, "gu_s", st) for _ in range(2)])
            OT = Ring([cx.sb([128, 512], BF16, "gu_o", st) for _ in range(3)])

            def jobs(g):
                col0 = g * NG

                def epi(tp0, g_, ct, accs, col0=col0):
                    r0 = col0 + ct * 128
                    for tt in range(len(accs[0])):
                        t0 = tp0 + tt * 512
                        s_ = ST.next()
                        o = OT.next()
                        p.op("act", lambda e, s_=s_, a=accs[0][tt]: e.activation(out=s_[:], in_=a[:], func=AF.Silu),
                             reads=[accs[0][tt].b], writes=[s_.b])
                        p.op("dve", lambda e, o=o, s_=s_, a=accs[1][tt]: e.tensor_tensor(out=o[:], in0=a[:], in1=s_[:], op=ALU.mult),
                             reads=[accs[1][tt].b, s_.b], writes=[o.b])
                        p.dma("pool", sc["act"][r0:r0 + 128, t0:t0 + 512], o[:], o.b, reads=[o.b], writes=[dbuf(nm("act"))])
                return [(w_gate_up[l], col0, 0), (w_gate_up[l], DFF + col0, 0)], epi
            phase_gemm("gu", [(sc["h2"], nm("h2_"))], D, TPg, NG, jobs, DFF // NG)
        if DFF % 256 == 0 and (DFF // 2) % 128 == 0 and S >= 1024:
            K2 = DFF // 2
            resid_gemm("wd1", sc["act"][0:K2, :], nm("act"), K2, 1024, 256, w_down[l][0:K2, :], sc["x1"], nm("x1_"), sc["xh"], nm("xh"))
            resid_gemm("wd2", sc["act"][K2:DFF, :], nm("act"), K2, 1024, 256, w_down[l][K2:DFF, :], sc["xh"], nm("xh"), sc["x2"], nm("x2_"))
        else:
            resid_gemm("wd", sc["act"], nm("act"), DFF, 512, 128, w_down[l], sc["x1"], nm("x1_"), sc["x2"], nm("x2_"))

    if c.get("cut", 99) < 99:
        phase_rmsnorm(xT_in, "xT_in", final_g, yT_out, "yT_out", out_f32=True)
    else:
        phase_rmsnorm(scr[L - 1]["x2"], f"x2_{L - 1}", final_g, yT_out, "yT_out", out_f32=True)
    top.close()
    return nc


def mixers(nc, p, cx, c, l, sc, nm, dbuf, K, W, QO, VO):
    D, S, NHD, NHM, NHB, BLK, TOPK = c["D"], c["S"], c["NHD"], c["NHM"], c["NHB"], c["BLK"], c["TOPK"]
    BW = NHD * 256
    NT = S // 128
    NB = S // BLK
    SW = S + 384
    ones_bf, ones_f, ident_f, utri_f, eps_t = K["ones_bf"], K["ones_f"], K["ident_f"], K["utri_f"], K["eps_t"]
    scale = 128 ** -0.5
    lam_init = 0.8 - 0.6 * math.exp(-0.3 * l)

    def common(st, need_strip=True):
        T = {}
        T["B1"] = cx.sb([128, SW], F32, "B1", st)
        B1 = T["B1"]
        p.op("pool", lambda e: e.iota(B1[:], pattern=[[1, SW]], base=-384, channel_multiplier=-1,
                                      allow_small_or_imprecise_dtypes=True), writes=[B1.b])
        if need_strip:
            T["NEG"] = cx.sb([128, SW], F32, "NEG", st)
            NEG = T["NEG"]
            p.op("dve", lambda e: e.tensor_scalar(out=NEG[:], in0=B1[:], scalar1=0.0, scalar2=1000.0, op0=ALU.min, op1=ALU.mult),
                 reads=[B1.b], writes=[NEG.b])
            T["STRIP"] = cx.sb([128, SW], F32, "STRIP", st)
        T["qT"] = cx.sb([128, S], BF16, "qT", st)
        T["kT"] = cx.sb([128, S], BF16, "kT", st)
        T["V"] = cx.sb([128, NT, 256], BF16, "V", st)
        T["SPS"] = Ring([cx.ps([128, 512], F32, "sps", st) for _ in range(3)])
        T["ACC"] = [cx.ps([128, 512], F32, "acc", st) for _ in range(3)]
        T["AUX"] = cx.ps([128, 512], F32, "aux", st)
        T["AUX2"] = cx.ps([128, 512], F32, "aux2", st)
        T["TT"] = Ring([cx.sb([128, 512], F32, "T", st) for _ in range(3)])
        T["PT"] = Ring([cx.sb([128, 512], BF16, "PT", st) for _ in range(5)])
        T["RR"] = cx.sb([128, 512], F32, "RR", st)
        T["O0"] = [cx.sb([128, 512], F32, "O0", st) for _ in range(2)]
        T["YY"] = [cx.sb([128, 512], F32, "YY", st) for _ in range(2)]
        T["YO"] = Ring([cx.sb([128, 512], BF16, "YO", st) for _ in range(3)])
        return T

    def run_pipe(jobs, lag=2):
        n = len(jobs)
        res = [None] * n
        for i in range(n + lag):
            if i < n:
                res[i] = jobs[i][0]()
            if i >= lag:
                jobs[i - lag][1](res[i - lag])

    def load_head(T, q_row, k_row, v_col, dv):
        qT, kT, V = T["qT"], T["kT"], T["V"]
        if q_row is not None:
            p.dma("sp", qT[:], sc["qk"][q_row:q_row + 128, :], qT.b, reads=[dbuf(nm("qk"))], writes=[qT.b])
        if k_row is not None:
            p.dma("sp", kT[:], sc["qk"][k_row:k_row + 128, :], kT.b, reads=[dbuf(nm("qk"))], writes=[kT.b])
        if v_col is not None:
            vv = sc["v"][:, v_col:v_col + dv].rearrange("(n p) d -> p n d", p=128)
            for n0 in range(0, NT, 8):
                n1 = min(NT, n0 + 8)
                p.dma("sp", V[:, n0:n1, 0:dv], vv[:, n0:n1, :], V.b, reads=[dbuf(nm("v"))], writes=[V.b])

    def make_strip(T, slope):
        B1, NEG, STRIP = T["B1"], T["NEG"], T["STRIP"]
        p.op("dve", lambda e: e.scalar_tensor_tensor(out=STRIP[:], in0=B1[:], scalar=-slope, in1=NEG[:], op0=ALU.mult, op1=ALU.add),
             reads=[B1.b, NEG.b], writes=[STRIP.b])

    def pv_accumulate(T, pt, kt, ndv, first, last, QW):
        ACC, V = T["ACC"], T["V"]
        for j in range(ndv):
            p.op("pe", lambda e, j=j, pt=pt, kt=kt: e.matmul(ACC[j][:, 0:QW], V[:, kt, j * 128:(j + 1) * 128], pt[:, 0:QW], start=first, stop=last),
                 reads=[V.b, pt.b], writes=[ACC[j].b])
        p.op("pe", lambda e, pt=pt: e.matmul(ACC[2][:, 0:QW], ones_bf[:], pt[:, 0:QW], start=first, stop=last),
             reads=[ones_bf.b, pt.b], writes=[ACC[2].b])

    def attn_tile(T, q0, QW, kt, pre_mask=None):
        qT, kT, STRIP = T["qT"], T["kT"], T["STRIP"]
        sp = T["SPS"].next()
        k0 = kt * 128
        p.op("pe", lambda e, sp=sp: e.matmul(sp[:, 0:QW], kT[:, k0:k0 + 128], qT[:, q0:q0 + QW], start=True, stop=(pre_mask is None)),
             reads=[kT.b, qT.b], writes=[sp.b])
        if pre_mask is not None:
            esel, n, nst = pre_mask
            p.op("pe", lambda e, sp=sp: e.matmul(sp[:, 0:QW], esel[0:16, n * 128:(n + 1) * 128], nst[0:16, 0:QW], start=False, stop=True),
                 reads=[nst.b, esel.b], writes=[sp.b])
        t = T["TT"].next()
        off = q0 - k0 + 384
        p.op("dve", lambda e, sp=sp, t=t: e.scalar_tensor_tensor(out=t[:, 0:QW], in0=sp[:, 0:QW], scalar=scale,
                                                                 in1=STRIP[:, off:off + QW], op0=ALU.mult, op1=ALU.add),
             reads=[sp.b, STRIP.b], writes=[t.b])
        pt = T["PT"].next()
        p.op("act", lambda e, t=t, pt=pt: e.activation(out=pt[:, 0:QW], in_=t[:, 0:QW], func=AF.Exp), reads=[t.b], writes=[pt.b])
        return pt

    with ExitStack() as st:
        T = common(st)
        ACC, AUX, RR, O0, YY, YO = T["ACC"], T["AUX"], T["RR"], T["O0"], T["YY"], T["YO"]
        C0 = [cx.sb([128, S], F32, "C0", st) for _ in range(2)]
        SQ = [cx.sb([128, 512], BF16, "SQb", st) for _ in range(2)]
        lam_t = cx.sb([128, 4], F32, "lam", st)
        dl = cx.sb([128, 4], F32, "dl", st)
        p.dma("sp", dl[:], W["diff_lambda"][l].rearrange("f d -> d f"), dl.b, writes=[dl.b], slow=True)
        pr = cx.sb([128, 2], F32, "pr", st)
        p.op("dve", lambda e: e.tensor_tensor(out=pr[:, 0:1], in0=dl[:, 0:1], in1=dl[:, 1:2], op=ALU.mult), reads=[dl.b], writes=[pr.b])
        p.op("dve", lambda e: e.tensor_tensor(out=pr[:, 1:2], in0=dl[:, 2:3], in1=dl[:, 3:4], op=ALU.mult), reads=[dl.b, pr.b], writes=[pr.b])
        p.op("pe", lambda e: e.matmul(AUX[:, 0:2], ones_f[:], pr[:], start=True, stop=True), reads=[ones_f.b, pr.b], writes=[AUX.b])
        p.op("act", lambda e: e.activation(out=lam_t[:, 0:2], in_=AUX[:, 0:2], func=AF.Exp), reads=[AUX.b], writes=[lam_t.b])
        p.op("dve", lambda e: e.tensor_tensor(out=lam_t[:, 2:3], in0=lam_t[:, 1:2], in1=lam_t[:, 0:1], op=ALU.subtract),
             reads=[lam_t.b], writes=[lam_t.b])
        p.op("dve", lambda e: e.tensor_scalar(out=lam_t[:, 2:3], in0=lam_t[:, 2:3], scalar1=-lam_init, scalar2=None, op0=ALU.add),
             reads=[lam_t.b], writes=[lam_t.b])
        gd = cx.sb([128, 2], F32, "gd", st)
        p.dma("sp", gd[:], W["diff_norm_g"][l:l + 1, :].rearrange("o (j p) -> p (o j)", p=128), gd.b, writes=[gd.b], slow=True)
        p.op("dve", lambda e: e.tensor_scalar(out=gd[:], in0=gd[:], scalar1=(1.0 - lam_init), scalar2=None, op0=ALU.mult),
             reads=[gd.b], writes=[gd.b])
        def diff_epilogue(q0, comp, h):
            p.op("dve", lambda e: e.reciprocal(out=RR[:], in_=ACC[2][:]), reads=[ACC[2].b], writes=[RR.b])
            for j in range(2):
                if comp == 0:
                    p.op("dve", lambda e, j=j, q0=q0: e.tensor_tensor(out=C0[j][:, q0:q0 + 512], in0=ACC[j][:], in1=RR[:], op=ALU.mult),
                         reads=[ACC[j].b, RR.b], writes=[C0[j].b])
                else:
                    p.op("dve", lambda e, j=j: e.tensor_tensor(out=O0[j][:], in0=ACC[j][:], in1=RR[:], op=ALU.mult),
                         reads=[ACC[j].b, RR.b], writes=[O0[j].b])
                    p.op("dve", lambda e, j=j, q0=q0: e.scalar_tensor_tensor(out=YY[j][:], in0=O0[j][:], scalar=lam_t[:, 2:3],
                                                                             in1=C0[j][:, q0:q0 + 512], op0=ALU.mult, op1=ALU.add),
                         reads=[O0[j].b, lam_t.b, C0[j].b], writes=[YY[j].b])
                    p.op("act", lambda e, j=j: e.activation(out=SQ[j][:], in_=YY[j][:], func=AF.Square), reads=[YY[j].b], writes=[SQ[j].b])
            if comp == 1:
                for j in range(2):
                    p.op("pe", lambda e, j=j: e.matmul(AUX[:], ones_bf[:], SQ[j][:], start=(j == 0), stop=(j == 1)),
                         reads=[ones_bf.b, SQ[j].b], writes=[AUX.b])
                p.op("act", lambda e: e.activation(out=RR[:], in_=AUX[:], func=AF.Sqrt, bias=eps_t[:, 0:1], scale=1.0 / 256),
                     reads=[AUX.b, eps_t.b], writes=[RR.b])
                p.op("dve", lambda e: e.reciprocal(out=RR[:], in_=RR[:]), reads=[RR.b], writes=[RR.b])
                for j in range(2):
                    yo = YO.next()
                    p.op("dve", lambda e, j=j, yo=yo: e.scalar_tensor_tensor(out=yo[:], in0=YY[j][:], scalar=gd[:, j:j + 1], in1=RR[:],
                                                                             op0=ALU.mult, op1=ALU.mult),
                         reads=[YY[j].b, gd.b, RR.b], writes=[yo.b])
                    r0 = h * 256 + j * 128
                    p.dma("pool", sc["y"][r0:r0 + 128, q0:q0 + 512], yo[:], yo.b, reads=[yo.b], writes=[dbuf(nm("y"))])

        for h in range(NHD):
            make_strip(T, 2.0 ** (-8.0 * (h + 1) / NHD))
            for comp in range(2):
                load_head(T, QO["dq"] + h * 256 + comp * 128, QO["dk"] + h * 256 + comp * 128,
                          VO["dv"] + h * 256 if comp == 0 else None, 256)
                jobs = []
                for qi in range(S // 512):
                    q0 = qi * 512
                    nkt = (q0 + 512) // 128
                    for kt in range(nkt):
                        def s1(q0=q0, kt=kt):
                            return attn_tile(T, q0, 512, kt)

                        def s2(pt, q0=q0, kt=kt, nkt=nkt, comp=comp, h=h):
                            pv_accumulate(T, pt, kt, 2, kt == 0, kt == nkt - 1, 512)
                            if kt == nkt - 1:
                                diff_epilogue(q0, comp, h)
                        jobs.append((s1, s2))
                run_pipe(jobs)
        p.end_phase()

    if c.get("cut", 99) <= 4:
        return
    with ExitStack() as st:
        T = common(st)
        ACC, AUX, AUX2, RR, YO, qT, kT = T["ACC"], T["AUX"], T["AUX2"], T["RR"], T["YO"], T["qT"], T["kT"]
        NBP = max(NB, 8)
        ESEL = cx.sb([16, 16 * 128], BF16, "ESEL", st)
        ETMP = cx.sb([16, 16 * 128], F32, "ETMP", st)
        p.op("pool", lambda e: e.iota(ETMP[:].rearrange("p (n m) -> p n m", m=128), pattern=[[1, 16], [0, 128]], base=0, channel_multiplier=-1,
                                      allow_small_or_imprecise_dtypes=True), writes=[ETMP.b])
        p.op("dve", lambda e: e.tensor_single_scalar(out=ESEL[:], in_=ETMP[:], scalar=0.0, op=ALU.is_equal), reads=[ETMP.b], writes=[ESEL.b])
        KM = cx.sb([128, 16], F32, "KM", st)
        QF = cx.sb([128, BLK], F32, "QF", st)
        GS = cx.sb([128, 16], F32, "GS", st)
        MX = cx.sb([128, 8], F32, "MX", st)
        NS = cx.sb([128, 16], F32, "NS", st)
        NST = cx.sb([16, BLK], BF16, "NST", st)
        for h in range(NHB):
            make_strip(T, 2.0 ** (-8.0 * (h + 1) / NHB))
            load_head(T, QO["bq"] + h * 128, QO["bk"] + h * 128, VO["bv"] + h * 128, 128)
            p.op("pool", lambda e: e.memset(KM[:], 0.0), writes=[KM.b])
            p.op("dve", lambda e: e.tensor_reduce(out=KM[:, 0:NB], in_=kT[:].rearrange("p (n k) -> p n k", k=BLK), axis=mybir.AxisListType.X, op=ALU.add),
                 reads=[kT.b, KM.b], writes=[KM.b])
            p.op("dve", lambda e: e.tensor_scalar(out=KM[:], in0=KM[:], scalar1=1.0 / BLK, scalar2=None, op0=ALU.mult), reads=[KM.b], writes=[KM.b])
            jobs = []
            for j in range(NB):
                q0 = j * BLK
                masked = j > TOPK

                def prep(j=j, q0=q0):
                    p.op("dve", lambda e, q0=q0: e.tensor_copy(out=QF[:], in_=qT[:, q0:q0 + BLK]), reads=[qT.b], writes=[QF.b])
                    for hf in range(BLK // 128):
                        p.op("pe", lambda e, hf=hf: e.matmul(AUX[:, 0:16], QF[:, hf * 128:(hf + 1) * 128], KM[:, 0:16], start=True, stop=True),
                             reads=[QF.b, KM.b], writes=[AUX.b])
                        p.op("pool", lambda e: e.memset(GS[:], -1e30), writes=[GS.b])
                        p.op("dve", lambda e, j=j: e.tensor_copy(out=GS[:, 0:j], in_=AUX[:, 0:j]), reads=[AUX.b, GS.b], writes=[GS.b])
                        p.op("dve", lambda e: e.max(out=MX[:], in_=GS[:]), reads=[GS.b], writes=[MX.b])
                        p.op("dve", lambda e: e.tensor_scalar(out=NS[:], in0=GS[:], scalar1=MX[:, TOPK - 1:TOPK], scalar2=-30000.0,
                                                              op0=ALU.is_lt, op1=ALU.mult), reads=[GS.b, MX.b], writes=[NS.b])
                        p.op("pe", lambda e: e.transpose(AUX2[0:16, 0:128], NS[:], ident_f[:]), reads=[NS.b, ident_f.b], writes=[AUX2.b])
                        p.op("act", lambda e, hf=hf: e.copy(out=NST[:, hf * 128:(hf + 1) * 128], in_=AUX2[0:16, 0:128]), reads=[AUX2.b], writes=[NST.b])
                def epi(q0=q0, h=h):
                    p.op("dve", lambda e: e.reciprocal(out=RR[:, 0:BLK], in_=ACC[2][:, 0:BLK]), reads=[ACC[2].b], writes=[RR.b])
                    yo = YO.next()
                    p.op("dve", lambda e, yo=yo: e.tensor_tensor(out=yo[:, 0:BLK], in0=ACC[0][:, 0:BLK], in1=RR[:, 0:BLK], op=ALU.mult),
                         reads=[ACC[0].b, RR.b], writes=[yo.b])
                    r0 = 2 * BW + h * 128
                    p.dma("pool", sc["y"][r0:r0 + 128, q0:q0 + BLK], yo[:, 0:BLK], yo.b, reads=[yo.b], writes=[dbuf(nm("y"))])

                nkt = (q0 + BLK) // 128
                for kt in range(nkt):
                    n = (kt * 128) // BLK
                    pm = (ESEL, n, NST) if (masked and n < j) else None

                    def s1(q0=q0, kt=kt, pm=pm, prep=prep, masked=masked):
                        if kt == 0 and masked:
                            prep()
                        return attn_tile(T, q0, BLK, kt, pm)

                    def s2(pt, kt=kt, nkt=nkt, epi=epi):
                        pv_accumulate(T, pt, kt, 1, kt == 0, kt == nkt - 1, BLK)
                        if kt == nkt - 1:
                            epi()
                    jobs.append((s1, s2))
            run_pipe(jobs)
        p.end_phase()

    if c.get("cut", 99) <= 5:
        return
    with ExitStack() as st:
        T = common(st, need_strip=False)
        ACC, AUX, AUX2, RR, O0, YY, YO, qT, kT, B1 = T["ACC"], T["AUX"], T["AUX2"], T["RR"], T["O0"], T["YY"], T["YO"], T["qT"], T["kT"], T["B1"]
        CM = cx.sb([128, SW], BF16, "CM", st)
        p.op("dve", lambda e: e.tensor_single_scalar(out=CM[:], in_=B1[:], scalar=0.0, op=ALU.is_ge), reads=[B1.b], writes=[CM.b])
        H2 = 2 * NHM
        GIF = cx.sb([128, NT, H2], F32, "GIF", st)
        GB = cx.sb([128, NT, H2], F32, "GB", st)
        gv = sc["gif"].rearrange("(n p) c -> p n c", p=128)
        for n0 in range(0, NT, 8):
            n1 = min(NT, n0 + 8)
            p.dma("sp", GIF[:, n0:n1, :], gv[:, n0:n1, :], GIF.b, reads=[dbuf(nm("gif"))], writes=[GIF.b], slow=True)
        gb_src = bass.AP(tensor=W["ml_gate_b"].tensor, offset=W["ml_gate_b"][l].offset, ap=[[0, 128], [0, NT], [1, H2]])
        p.dma("sp", GB[:], gb_src, GB.b, writes=[GB.b], slow=True)
        p.op("dve", lambda e: e.tensor_tensor(out=GIF[:], in0=GIF[:], in1=GB[:], op=ALU.add), reads=[GIF.b, GB.b], writes=[GIF.b])
        LF = cx.sb([128, NT, NHM], F32, "LF", st)
        p.op("act", lambda e: e.activation(out=LF[:], in_=GIF[:, :, NHM:H2], func=AF.Exp, scale=-1.0), reads=[GIF.b], writes=[LF.b])
        p.op("act", lambda e: e.activation(out=LF[:], in_=LF[:], func=AF.Ln, bias=eps_t[:, 1:2], scale=1.0), reads=[LF.b, eps_t.b], writes=[LF.b])
        p.op("dve", lambda e: e.tensor_scalar(out=LF[:], in0=LF[:], scalar1=-1.0, scalar2=None, op0=ALU.mult), reads=[LF.b], writes=[LF.b])
        NC_ = NT * NHM
        G = cx.sb([128, NT, NHM], F32, "G", st)
        TOTS = cx.sb([128, NT, NHM], F32, "TOTS", st)
        PRE = cx.sb([128, NT, NHM], F32, "PRE", st)
        U = cx.sb([128, NT, NHM], F32, "U", st)
        for c0 in range(0, NC_, 512):
            c1 = min(NC_, c0 + 512)
            lf2 = LF[:].rearrange("p n h -> p (n h)")
            p.op("pe", lambda e, c0=c0, c1=c1, lf2=lf2: e.matmul(AUX[:, 0:c1 - c0], utri_f[:], lf2[:, c0:c1], start=True, stop=True),
                 reads=[utri_f.b, LF.b], writes=[AUX.b])
            p.op("pe", lambda e, c0=c0, c1=c1, lf2=lf2: e.matmul(AUX2[:, 0:c1 - c0], ones_f[:], lf2[:, c0:c1], start=True, stop=True),
                 reads=[ones_f.b, LF.b], writes=[AUX2.b])
            p.op("dve", lambda e, c0=c0, c1=c1: e.tensor_copy(out=G[:].rearrange("p n h -> p (n h)")[:, c0:c1], in_=AUX[:, 0:c1 - c0]),
                 reads=[AUX.b], writes=[G.b])
            p.op("dve", lambda e, c0=c0, c1=c1: e.tensor_copy(out=TOTS[:].rearrange("p n h -> p (n h)")[:, c0:c1], in_=AUX2[:, 0:c1 - c0]),
                 reads=[AUX2.b], writes=[TOTS.b])
        p.op("pool", lambda e: e.memset(PRE[:], 0.0), writes=[PRE.b])
        for n in range(1, NT):
            p.op("dve", lambda e, n=n: e.tensor_tensor(out=PRE[:, n, :], in0=PRE[:, n - 1, :], in1=TOTS[:, n - 1, :], op=ALU.add),
                 reads=[PRE.b, TOTS.b], writes=[PRE.b])
        p.op("dve", lambda e: e.tensor_tensor(out=G[:], in0=G[:], in1=PRE[:], op=ALU.add), reads=[G.b, PRE.b], writes=[G.b])
        p.op("dve", lambda e: e.tensor_tensor(out=U[:], in0=GIF[:, :, 0:NHM], in1=G[:], op=ALU.subtract), reads=[GIF.b, G.b], writes=[U.b])
        GBC = cx.sb([128, S], F32, "GBC", st)
        DG = Ring([cx.sb([128, 128], F32, "DG", st) for _ in range(2)])
        XP = cx.sb([128, S + 3], BF16, "XP", st)
        CA = cx.sb([128, S], F32, "CA", st)
        cw = cx.sb([128, 4], F32, "cw", st)
        cb = cx.sb([128, 1], F32, "cb", st)
        gm = cx.sb([128, 2], F32, "gm", st)
        DT = Ring([cx.sb([128, 512], F32, "DT", st) for _ in range(2)])
        OG = Ring([cx.sb([128, 512], BF16, "OG", st) for _ in range(2)])
        SQF = [cx.sb([128, 512], F32, "SQf", st) for _ in range(2)]
        MEAN = cx.sb([128, 512], F32, "MEAN", st)
        p.op("pool", lambda e: e.memset(XP[:, 0:3], 0.0), writes=[XP.b])

        def conv_silu(row, ch0, dst):
            p.dma("sp", XP[:, 3:S + 3], sc["qk"][row:row + 128, :], XP.b, reads=[dbuf(nm("qk"))], writes=[XP.b])
            p.dma("sp", cw[:], W["ml_conv_w"][l][:, ch0:ch0 + 128].rearrange("j c -> c j"), cw.b, writes=[cw.b], slow=True)
            p.dma("sp", cb[:], W["ml_conv_b"][l:l + 1, ch0:ch0 + 128].rearrange("o c -> c o"), cb.b, writes=[cb.b], slow=True)
            p.op("dve", lambda e: e.tensor_scalar(out=CA[:], in0=XP[:, 0:S], scalar1=cw[:, 0:1], scalar2=None, op0=ALU.mult),
                 reads=[XP.b, cw.b], writes=[CA.b])
            for jj in range(1, 4):
                p.op("dve", lambda e, jj=jj: e.scalar_tensor_tensor(out=CA[:], in0=XP[:, jj:jj + S], scalar=cw[:, jj:jj + 1], in1=CA[:],
                                                                    op0=ALU.mult, op1=ALU.add), reads=[XP.b, cw.b, CA.b], writes=[CA.b])
            p.op("act", lambda e: e.activation(out=dst[:], in_=CA[:], func=AF.Silu, bias=cb[:, 0:1], scale=1.0), reads=[CA.b, cb.b], writes=[dst.b])

        def ml_tile(q0, kt, h):
            k0 = kt * 128
            sp = T["SPS"].next()
            p.op("pe", lambda e, sp=sp, k0=k0, q0=q0: e.matmul(sp[:], kT[:, k0:k0 + 128], qT[:, q0:q0 + 512], start=True, stop=True),
                 reads=[kT.b, qT.b], writes=[sp.b])
            dt = DT.next()
            p.op("act", lambda e, dt=dt, q0=q0, kt=kt, h=h: e.activation(out=dt[:], in_=GBC[:, q0:q0 + 512], func=AF.Exp,
                                                                         bias=U[:, kt, h:h + 1], scale=1.0), reads=[GBC.b, U.b], writes=[dt.b])
            pt = T["PT"].next()
            if k0 + 128 > q0:
                off = q0 - k0 + 384
                p.op("pool", lambda e, dt=dt, off=off: e.tensor_tensor(out=dt[:], in0=dt[:], in1=CM[:, off:off + 512], op=ALU.mult),
                     reads=[dt.b, CM.b], writes=[dt.b])
            p.op("dve", lambda e, sp=sp, dt=dt, pt=pt: e.scalar_tensor_tensor(out=pt[:], in0=sp[:], scalar=scale, in1=dt[:], op0=ALU.mult, op1=ALU.mult),
                 reads=[sp.b, dt.b], writes=[pt.b])
            return pt

        def ml_epilogue(q0, h):
            p.op("act", lambda e: e.activation(out=RR[:], in_=ACC[2][:], func=AF.Abs), reads=[ACC[2].b], writes=[RR.b])
            p.op("dve", lambda e: e.tensor_scalar_max(out=RR[:], in0=RR[:], scalar1=1.0), reads=[RR.b], writes=[RR.b])
            p.op("dve", lambda e: e.reciprocal(out=RR[:], in_=RR[:]), reads=[RR.b], writes=[RR.b])
            for j in range(2):
                p.op("dve", lambda e, j=j: e.tensor_tensor(out=O0[j][:], in0=ACC[j][:], in1=RR[:], op=ALU.mult),
                     reads=[ACC[j].b, RR.b], writes=[O0[j].b])
                p.op("act", lambda e, j=j: e.activation(out=SQF[j][:], in_=O0[j][:], func=AF.Square), reads=[O0[j].b], writes=[SQF[j].b])
            for j in range(2):
                p.op("pe", lambda e, j=j: e.matmul(AUX[:], ones_f[:], O0[j][:], start=(j == 0), stop=(j == 1)),
                     reads=[ones_f.b, O0[j].b], writes=[AUX.b])
            for j in range(2):
                p.op("pe", lambda e, j=j: e.matmul(AUX2[:], ones_f[:], SQF[j][:], start=(j == 0), stop=(j == 1)),
                     reads=[ones_f.b, SQF[j].b], writes=[AUX2.b])
            p.op("dve", lambda e: e.tensor_scalar(out=MEAN[:], in0=AUX[:], scalar1=1.0 / 256, scalar2=None, op0=ALU.mult), reads=[AUX.b], writes=[MEAN.b])
            p.op("dve", lambda e: e.tensor_tensor(out=RR[:], in0=MEAN[:], in1=MEAN[:], op=ALU.mult), reads=[MEAN.b], writes=[RR.b])
            p.op("dve", lambda e: e.scalar_tensor_tensor(out=RR[:], in0=AUX2[:], scalar=1.0 / 256, in1=RR[:], op0=ALU.mult, op1=ALU.subtract),
                 reads=[AUX2.b, RR.b], writes=[RR.b])
            p.op("act", lambda e: e.activation(out=RR[:], in_=RR[:], func=AF.Sqrt, bias=eps_t[:, 0:1], scale=1.0), reads=[RR.b, eps_t.b], writes=[RR.b])
            p.op("dve", lambda e: e.reciprocal(out=RR[:], in_=RR[:]), reads=[RR.b], writes=[RR.b])
            for j in range(2):
                og = OG.next()
                r0 = QO["mo"] + h * 256 + j * 128
                p.dma("sp", og[:], sc["qk"][r0:r0 + 128, q0:q0 + 512], og.b, reads=[dbuf(nm("qk"))], writes=[og.b])
                p.op("dve", lambda e, j=j: e.tensor_tensor(out=YY[j][:], in0=O0[j][:], in1=MEAN[:], op=ALU.subtract), reads=[O0[j].b, MEAN.b], writes=[YY[j].b])
                p.op("dve", lambda e, j=j: e.scalar_tensor_tensor(out=YY[j][:], in0=YY[j][:], scalar=gm[:, j:j + 1], in1=RR[:], op0=ALU.mult, op1=ALU.mult),
                     reads=[YY[j].b, gm.b, RR.b], writes=[YY[j].b])
                yo = YO.next()
                p.op("dve", lambda e, j=j, yo=yo, og=og: e.tensor_tensor(out=yo[:], in0=YY[j][:], in1=og[:], op=ALU.mult), reads=[YY[j].b, og.b], writes=[yo.b])
                r1 = BW + h * 256 + j * 128
                p.dma("pool", sc["y"][r1:r1 + 128, q0:q0 + 512], yo[:], yo.b, reads=[yo.b], writes=[dbuf(nm("y"))])

        for h in range(NHM):
            conv_silu(QO["mq"] + h * 128, h * 128, qT)
            conv_silu(QO["mk"] + h * 128, NHM * 128 + h * 128, kT)
            load_head(T, None, None, VO["mv"] + h * 256, 256)
            p.dma("sp", gm[:], W["ml_norm_g"][l:l + 1, h * 256:(h + 1) * 256].rearrange("o (j p) -> p (o j)", p=128), gm.b, writes=[gm.b], slow=True)
            for n in range(NT):
                dg = DG.next()
                p.op("dve", lambda e, dg=dg, n=n, h=h: e.tensor_scalar(out=dg[:], in0=ident_f[:], scalar1=G[:, n, h:h + 1], scalar2=None, op0=ALU.mult),
                     reads=[ident_f.b, G.b], writes=[dg.b])
                p.op("pe", lambda e, dg=dg, n=n: e.matmul(AUX[:, (n % 4) * 128:(n % 4 + 1) * 128], ones_f[:], dg[:], start=True, stop=True),
                     reads=[ones_f.b, dg.b], writes=[AUX.b])
                if n % 4 == 3 or n == NT - 1:
                    n0 = (n // 4) * 4
                    w = (n - n0 + 1) * 128
                    p.op("act", lambda e, n0=n0, w=w: e.copy(out=GBC[:, n0 * 128:n0 * 128 + w], in_=AUX[:, 0:w]), reads=[AUX.b], writes=[GBC.b])
            jobs = []
            for qi in range(S // 512):
                q0 = qi * 512
                nkt = (q0 + 512) // 128
                for kt in range(nkt):
                    def s1(q0=q0, kt=kt, h=h):
                        return ml_tile(q0, kt, h)

                    def s2(pt, q0=q0, kt=kt, nkt=nkt, h=h):
                        pv_accumulate(T, pt, kt, 2, kt == 0, kt == nkt - 1, 512)
                        if kt == nkt - 1:
                            ml_epilogue(q0, h)
                    jobs.append((s1, s2))
            run_pipe(jobs)
        p.end_phase()


_CACHE = {}


REAL_CORES = (0, 1, 4, 5)


def kernel(**inputs):
    c = cfg_full()
    return run_cfg(c, inputs, n_cores=4, spread=True)


def run_cfg(c, inputs, n_cores, spread=False):
    key = tuple(sorted(c.items()))
    if key not in _CACHE:
        _CACHE[key] = build(c)
    nc = _CACHE[key]
    x = np.asarray(inputs["x"])
    B = x.shape[0]
    assert B == n_cores
    shared = {k: np.ascontiguousarray(np.asarray(v)) for k, v in inputs.items() if k != "x"}
    shared["final_g"] = shared["final_g"].reshape(1, -1)
    in_maps = []
    for b in range(B):
        m = dict(shared)
        m["xT"] = np.ascontiguousarray(x[b].T)
        in_maps.append(m)
    slots = list(range(B))
    if spread:
        zero = {k: np.zeros_like(v) for k, v in in_maps[0].items()}
        full = [zero] * 8
        full = list(full)
        for b, cidx in enumerate(REAL_CORES):
            full[cidx] = in_maps[b]
        in_maps = full
        slots = list(REAL_CORES)
    res = run_bass_kernel_spmd(nc, in_maps, core_ids=list(range(len(in_maps))))
    out = np.stack([np.ascontiguousarray(res.results[slots[b]]["yT"].T) for b in range(B)], axis=0)
    return out.astype(np.float32)
```

```python
import math
from contextlib import ExitStack
import numpy as np
import concourse.bass as bass
import concourse.mybir as mybir
from concourse.bass_utils import run_bass_kernel_spmd

F32 = mybir.dt.float32
BF16 = mybir.dt.bfloat16
AF = mybir.ActivationFunctionType
ALU = mybir.AluOpType
EPS = 1e-6
SAME_ENG_SYNC = True


_UID = [0]


class Buf:
    def __init__(self, name, loose=False):
        _UID[0] += 1
        self.uid = _UID[0]
        self.name = name
        self.w = {}
        self.r = {}
        self.sem = None
        self.semv = 0
        self.loose = loose


class Ins:
    __slots__ = ("eng", "fn", "waits", "sig", "cnt", "idx", "dbuf")

    def __init__(self, eng, fn, idx):
        self.eng, self.fn, self.idx = eng, fn, idx
        self.waits = []
        self.sig = False
        self.cnt = 0
        self.dbuf = None


ENGS = ("pe", "act", "dve", "pool", "sp")


class Prog:
    def __init__(self, nc, stack):
        self.nc = nc
        self.stack = stack
        self.q = {e: [] for e in ENGS}
        self.known = {e: {} for e in ENGS}
        self.dbufs = []
        self.esem = {}
        self.nidx = {e: 0 for e in ENGS}
        self.ccnt = {e: 0 for e in ENGS}
        self.last = {e: None for e in ENGS}
        self.sempool = []
        self.nsem = 0
        for e in ENGS:
            self.esem[e] = self.stack.enter_context(nc.semaphore("e_" + e))

    def _new(self, eng, fn):
        ins = Ins(eng, fn, self.nidx[eng])
        self.nidx[eng] += 1
        self.q[eng].append(ins)
        return ins

    def _deps(self, eng, reads, writes, is_dma):
        deps = {}

        def add(ev):
            if ev[0] == "E":
                key, order = ev[1].eng, ev[1].idx
            else:
                key, order = ev[1].uid, ev[2]
            if key not in deps or deps[key][1] < order:
                deps[key] = (ev, order)

        for b in reads:
            for ev in b.w.values():
                add(ev)
        for b in writes:
            if b.loose:
                continue
            for ev in b.w.values():
                add(ev)
            for ev in b.r.values():
                add(ev)
        out = []
        for key, (ev, order) in deps.items():
            if ev[0] == "E" and ev[1].eng == eng:
                if eng in ("pe", "sp") or is_dma or not SAME_ENG_SYNC:
                    continue
            if self.known[eng].get(key, -1) >= order:
                continue
            self.known[eng][key] = order
            out.append(ev)
        return out

    def op(self, eng, fn, reads=(), writes=()):
        ins = self._new(eng, fn)
        self.last[eng] = ins
        ins.waits = self._deps(eng, reads, writes, False)
        ev = ("E", ins)
        for b in reads:
            b.r[eng] = ev
        for b in writes:
            b.w = {eng: ev}
            b.r = {}
        return ins

    def dma(self, eng, out, in_, sb, reads=(), writes=(), slow=False):
        if sb.sem is None:
            if self.sempool:
                sb.sem, sb.semv = self.sempool.pop()
            else:
                self.nsem += 1
                sb.sem, sb.semv = self.stack.enter_context(self.nc.semaphore(f"d_{self.nsem}")), 0
            self.dbufs.append(sb)
        if slow:
            fn = lambda e: e.dma_start(out=out, in_=in_, allow_slow_non_contiguous=True)
        else:
            fn = lambda e: e.dma_start(out=out, in_=in_)
        ins = self._new(eng, fn)
        ins.waits = self._deps(eng, reads, writes, True)
        ins.dbuf = sb
        sb.semv += 16
        ev = ("D", sb, sb.semv)
        key = sb.uid
        for b in reads:
            if not b.loose:
                b.r[key] = ev
        for b in writes:
            if b.loose:
                b.w[key] = ev
            else:
                b.w = {key: ev}
                b.r = {}
        return ins

    def end_phase(self):
        last = dict(self.last)
        dsnap = [(b, b.semv) for b in self.dbufs]
        for e in ENGS:
            ins = self._new(e, None)
            for e2 in ENGS:
                if e2 != e and last[e2] is not None:
                    if self.known[e].get(e2, -1) < last[e2].idx:
                        self.known[e][e2] = last[e2].idx
                        ins.waits.append(("E", last[e2]))
            for b, v in dsnap:
                if self.known[e].get(b.uid, -1) < v:
                    self.known[e][b.uid] = v
                    ins.waits.append(("D", b, v))
        self.flush()
        for b in self.dbufs:
            self.sempool.append((b.sem, b.semv))
            b.sem = None
        self.dbufs = []

    def flush(self):
        nc = self.nc
        for e in ENGS:
            for ins in self.q[e]:
                for ev in ins.waits:
                    if ev[0] == "E":
                        ev[1].sig = True
        for e in ENGS:
            c = self.ccnt[e]
            for ins in self.q[e]:
                if ins.sig:
                    assert ins.fn is not None
                    c += 1
                ins.cnt = c
            self.ccnt[e] = c
        esem = self.esem
        qs = self.q
        self.q = {e: [] for e in ENGS}

        def run(e, eng):
            for ins in qs[e]:
                for ev in ins.waits:
                    if ev[0] == "E":
                        eng.wait_ge(esem[ev[1].eng], ev[1].cnt)
                    else:
                        eng.wait_ge(ev[1].sem, ev[2])
                if ins.fn is None:
                    continue
                r = ins.fn(eng)
                if ins.dbuf is not None:
                    r.then_inc(ins.dbuf.sem, 16)
                elif ins.sig:
                    r.then_inc(esem[e], 1)

        with nc.Block() as block:
            @block.tensor
            def _(eng):
                run("pe", eng)

            @block.scalar
            def _(eng):
                run("act", eng)

            @block.vector
            def _(eng):
                run("dve", eng)

            @block.gpsimd
            def _(eng):
                run("pool", eng)

            @block.sync
            def _(eng):
                run("sp", eng)


class Tile:
    def __init__(self, t, name):
        self.t = t
        self.b = Buf(name)

    def __getitem__(self, k):
        return self.t[k]


class Ctx:
    def __init__(self, nc, p, stack):
        self.nc, self.p, self.stack = nc, p, stack
        self.n = 0

    def sb(self, shape, dt, name, stack=None):
        self.n += 1
        t = (stack or self.stack).enter_context(self.nc.sbuf_tensor(f"{name}_{self.n}", list(shape), dt))
        return Tile(t, f"{name}_{self.n}")

    def ps(self, shape, dt, name, stack=None):
        self.n += 1
        t = (stack or self.stack).enter_context(self.nc.psum_tensor(f"{name}_{self.n}", list(shape), dt))
        return Tile(t, f"{name}_{self.n}")


class Ring:
    def __init__(self, tiles):
        self.tiles = tiles
        self.i = 0

    def next(self):
        t = self.tiles[self.i % len(self.tiles)]
        self.i += 1
        return t


def cfg_full():
    return dict(D=4096, S=4096, L=2, NHD=8, NHM=8, NHB=16, DFF=11008, BLK=256, TOPK=3)


def col_offsets(c):
    D, NHD, NHM, NHB = c["D"], c["NHD"], c["NHM"], c["NHB"]
    sizes = [NHD * 256, NHD * 256, NHD * 256, NHM * 128, NHM * 128, NHM * 256, NHM * 256, NHM, NHM,
             NHB * 128, NHB * 128, NHB * 128, 3 * D]
    names = ["dq", "dk", "dv", "mq", "mk", "mv", "mo", "mi", "mf", "bq", "bk", "bv", "gates"]
    off = {}
    o = 0
    for n, s in zip(names, sizes):
        off[n] = o
        o += s
    off["_total"] = o
    return off


def build(c):
    D, S, L, NHD, NHM, NHB, DFF = c["D"], c["S"], c["L"], c["NHD"], c["NHM"], c["NHB"], c["DFF"]
    BLK, TOPK = c["BLK"], c["TOPK"]
    KC = D // 128
    BW = NHD * 256
    assert BW == NHM * 256 == NHB * 128
    OFF = col_offsets(c)
    DIN = OFF["_total"]
    NT = S // 128
    NB = S // BLK
    SW = S + 384

    nc = bass.Bass("TRN2", target_bir_lowering=False)

    def din(name, shape):
        return nc.dram_tensor(name, list(shape), F32, kind="ExternalInput").ap()

    xT_in = din("xT", [D, S])
    norm1_g = din("norm1_g", [L, D])
    w_in = din("w_in", [L, D, DIN])
    diff_lambda = din("diff_lambda", [L, 4, 128])
    diff_norm_g = din("diff_norm_g", [L, 256])
    ml_conv_w = din("ml_conv_w", [L, 4, 2 * NHM * 128])
    ml_conv_b = din("ml_conv_b", [L, 2 * NHM * 128])
    ml_gate_b = din("ml_gate_b", [L, 2, NHM])
    ml_norm_g = din("ml_norm_g", [L, NHM * 256])
    w_branch = din("w_branch", [L, 3, BW, D])
    w_out = din("w_out", [L, D, D])
    norm2_g = din("norm2_g", [L, D])
    w_gate_up = din("w_gate_up", [L, D, 2 * DFF])
    w_down = din("w_down", [L, DFF, D])
    final_g = din("final_g", [1, D])
    yT_out = nc.dram_tensor("yT", [D, S], F32, kind="ExternalOutput").ap()

    def scratch(name, shape, dt):
        return nc.dram_tensor(name, list(shape), dt, kind="Internal").ap()

    NQK = 2 * NHD * 256 + 2 * NHM * 128 + NHM * 256 + 2 * NHB * 128
    NV = NHD * 256 + NHM * 256 + NHB * 128
    scr = []
    for l in range(L):
        scr.append(dict(
            hT=scratch(f"hT{l}", [D, S], BF16), qk=scratch(f"qk{l}", [NQK, S], BF16),
            v=scratch(f"v{l}", [S, NV], BF16), gif=scratch(f"gif{l}", [S, 2 * NHM], F32),
            gates=scratch(f"gates{l}", [3 * D, S], BF16), y=scratch(f"y{l}", [3 * BW, S], BF16),
            merged=scratch(f"merged{l}", [D, S], BF16), x1=scratch(f"x1_{l}", [D, S], F32),
            h2=scratch(f"h2_{l}", [D, S], BF16), act=scratch(f"act{l}", [DFF, S], BF16),
            x2=scratch(f"x2_{l}", [D, S], F32), xh=scratch(f"xh{l}", [D, S], F32)))
    QO = {}
    o = 0
    for n, s in (("dq", NHD * 256), ("dk", NHD * 256), ("mq", NHM * 128), ("mk", NHM * 128), ("mo", NHM * 256),
                 ("bq", NHB * 128), ("bk", NHB * 128)):
        QO[n] = o
        o += s
    VO = {"dv": 0, "mv": NHD * 256, "bv": NHD * 256 + NHM * 256}

    top = ExitStack()
    p = Prog(nc, top)
    cx = Ctx(nc, p, top)
    dram = {}

    def dbuf(ap_name):
        if ap_name not in dram:
            dram[ap_name] = Buf(ap_name, loose=True)
        return dram[ap_name]

    ones_bf = cx.sb([128, 128], BF16, "ones_bf")
    ones_f = cx.sb([128, 128], F32, "ones_f")
    ident_f = cx.sb([128, 128], F32, "ident_f")
    utri_f = cx.sb([128, 128], F32, "utri_f")
    iot = cx.sb([128, 128], F32, "iot")
    p.op("pool", lambda e: e.memset(ones_bf[:], 1.0), writes=[ones_bf.b])
    p.op("pool", lambda e: e.memset(ones_f[:], 1.0), writes=[ones_f.b])
    eps_t = cx.sb([128, 2], F32, "eps_t")
    p.op("pool", lambda e: e.memset(eps_t[:, 0:1], EPS), writes=[eps_t.b])
    p.op("pool", lambda e: e.memset(eps_t[:, 1:2], 1.0), writes=[eps_t.b])
    p.op("pool", lambda e: e.iota(iot[:], pattern=[[1, 128]], base=0, channel_multiplier=-1,
                                  allow_small_or_imprecise_dtypes=True), writes=[iot.b])
    p.op("dve", lambda e: e.tensor_single_scalar(out=ident_f[:], in_=iot[:], scalar=0.0, op=ALU.is_equal),
         reads=[iot.b], writes=[ident_f.b])
    p.op("dve", lambda e: e.tensor_single_scalar(out=utri_f[:], in_=iot[:], scalar=0.0, op=ALU.is_ge),
         reads=[iot.b], writes=[utri_f.b])
    p.end_phase()

    def phase_rmsnorm(src, src_name, g_ap, dst, dst_name, out_f32=False):
        TT = 256
        with ExitStack() as st:
            gt = cx.sb([128, KC], F32, "nrm_g", st)
            p.dma("sp", gt[:], g_ap.rearrange("o (c p) -> p (o c)", p=128), gt.b, writes=[gt.b], slow=True)
            X = [cx.sb([128, KC, TT], F32, "nrm_x", st) for _ in range(2)]
            SQ = cx.sb([128, KC, TT], BF16, "nrm_sq", st)
            H = [cx.sb([128, KC, TT], F32 if out_f32 else BF16, "nrm_h", st) for _ in range(2)]
            R = cx.sb([128, TT], F32, "nrm_r", st)
            PS = cx.ps([128, TT], F32, "nrm_ps", st)
            sview = src.rearrange("(c p) t -> p c t", p=128)
            dview = dst.rearrange("(c p) t -> p c t", p=128)
            n = S // TT
            for i in range(n):
                x = X[i % 2]
                h = H[i % 2]
                for k0 in range(0, KC, 8):
                    k1 = min(KC, k0 + 8)
                    p.dma("sp", x[:, k0:k1, :], sview[:, k0:k1, i * TT:(i + 1) * TT], x.b, reads=[dbuf(src_name)], writes=[x.b])
                p.op("act", lambda e, x=x: e.activation(out=SQ[:], in_=x[:], func=AF.Square), reads=[x.b], writes=[SQ.b])
                for k in range(KC):
                    p.op("pe", lambda e, k=k: e.matmul(PS[:], ones_bf[:], SQ[:, k, :], start=(k == 0), stop=(k == KC - 1)),
                         reads=[SQ.b, ones_bf.b], writes=[PS.b])
                p.op("act", lambda e: e.activation(out=R[:], in_=PS[:], func=AF.Sqrt, bias=eps_t[:, 0:1], scale=1.0 / D),
                     reads=[PS.b, eps_t.b], writes=[R.b])
                p.op("dve", lambda e: e.reciprocal(out=R[:], in_=R[:]), reads=[R.b], writes=[R.b])
                for k in range(KC):
                    p.op("dve", lambda e, k=k, x=x, h=h: e.scalar_tensor_tensor(out=h[:, k, :], in0=x[:, k, :], scalar=gt[:, k:k + 1],
                                                                               in1=R[:], op0=ALU.mult, op1=ALU.mult),
                         reads=[x.b, R.b, gt.b], writes=[h.b])
                for k0 in range(0, KC, 8):
                    k1 = min(KC, k0 + 8)
                    p.dma("pool", dview[:, k0:k1, i * TT:(i + 1) * TT], h[:, k0:k1, :], h.b, reads=[h.b], writes=[dbuf(dst_name)])
            p.end_phase()

    def phase_gemm(name, acts, K, TP, NG, jobs_for_group, ncolgroups, tokmajor=False, nws=4):
        KCk = (K + 127) // 128
        assert K % 128 == 0
        PIECE = 8
        with ExitStack() as st:
            A = [cx.sb([128, KCk, TP], BF16, f"{name}_a{i}", st) for i in range(len(acts))]
            nacc = len(jobs_for_group(0)[0])
            WST = Ring([cx.sb([128, PIECE, NG], F32, f"{name}_ws", st) for _ in range(nws)])
            WSL = [Ring([cx.sb([128, KCk, NG], BF16, f"{name}_wb{a}", st) for _ in range(2)]) for a in range(nacc)]
            NTT = TP // 512 if not tokmajor else TP // 128
            banks = Ring([cx.ps([128, 512], F32, f"{name}_ps", st) for _ in range(8)])
            cast_i = [0]

            def load_group(g):
                wspecs, epi = jobs_for_group(g)
                slabs = []
                for a, (w2d, col0, ai) in enumerate(wspecs):
                    slab = WSL[a].next()
                    wv = w2d.rearrange("(c p) n -> p c n", p=128)
                    for k0 in range(0, KCk, PIECE):
                        k1 = min(KCk, k0 + PIECE)
                        ws = WST.next()
                        p.dma("sp", ws[:, 0:k1 - k0, :], wv[:, k0:k1, col0:col0 + NG], ws.b, writes=[ws.b])
                        eng = ("dve", "act")[cast_i[0] % 2]
                        cast_i[0] += 1
                        if eng == "dve":
                            p.op("dve", lambda e, ws=ws, slab=slab, k0=k0, k1=k1: e.tensor_copy(out=slab[:, k0:k1, :], in_=ws[:, 0:k1 - k0, :]),
                                 reads=[ws.b], writes=[slab.b])
                        else:
                            p.op("act", lambda e, ws=ws, slab=slab, k0=k0, k1=k1: e.copy(out=slab[:, k0:k1, :], in_=ws[:, 0:k1 - k0, :]),
                                 reads=[ws.b], writes=[slab.b])
                    slabs.append(slab)
                return wspecs, epi, slabs

            work = [(tp0, g) for tp0 in range(0, S, TP) for g in range(ncolgroups)]
            nxt = load_group(work[0][1])
            for wi, (tp0, g) in enumerate(work):
                if g == 0:
                    for i, (a_ap, a_name) in enumerate(acts):
                        av = a_ap.rearrange("(c p) t -> p c t", p=128)
                        for k0 in range(0, KCk, 16):
                            k1 = min(KCk, k0 + 16)
                            p.dma("sp", A[i][:, k0:k1, :], av[:, k0:k1, tp0:tp0 + TP], A[i].b, reads=[dbuf(a_name)], writes=[A[i].b])
                wspecs, epi, slabs = nxt
                if wi + 1 < len(work):
                    nxt = load_group(work[wi + 1][1])
                if not tokmajor:
                    for ct in range(NG // 128):
                        accs = []
                        for a, (w2d, col0, ai) in enumerate(wspecs):
                            tiles = [banks.next() for _ in range(NTT)]
                            for k in range(KCk):
                                for tt in range(NTT):
                                    p.op("pe", lambda e, a=a, k=k, tt=tt, ct=ct, ai=ai, tl=tiles[tt], sl=slabs[a]:
                                         e.matmul(tl[:], sl[:, k, ct * 128:(ct + 1) * 128], A[ai][:, k, tt * 512:(tt + 1) * 512],
                                                  start=(k == 0), stop=(k == KCk - 1)),
                                         reads=[slabs[a].b, A[ai].b], writes=[tiles[tt].b])
                            accs.append(tiles)
                        epi(tp0, g, ct, accs)
                else:
                    for tt in range(NTT):
                        tl = banks.next()
                        for k in range(KCk):
                            p.op("pe", lambda e, k=k, tt=tt, tl=tl, sl=slabs[0]:
                                 e.matmul(tl[:, 0:NG], A[0][:, k, tt * 128:(tt + 1) * 128], sl[:, k, :],
                                          start=(k == 0), stop=(k == KCk - 1)),
                                 reads=[slabs[0].b, A[0].b], writes=[tl.b])
                        epi(tp0, g, tt, tl)
            p.end_phase()

    for l in range(L):
        sc = scr[l]
        x_src, x_src_name = (xT_in, "xT_in") if l == 0 else (scr[l - 1]["x2"], f"x2_{l - 1}")
        nm = lambda s: f"{s}{l}"
        CUT = c.get("cut", 99)
        phase_rmsnorm(x_src, x_src_name, norm1_g[l:l + 1, :], sc["hT"], nm("hT"))
        if CUT <= 1:
            break

        with ExitStack() as st:
            NG = 256 if (NHM * 128) % 256 == 0 else 128
            OST = Ring([cx.sb([128, 512], BF16, "zin_o", st) for _ in range(4)])
            groups = []
            for n_, sz in (("dq", NHD * 256), ("dk", NHD * 256), ("mq", NHM * 128), ("mk", NHM * 128), ("mo", NHM * 256),
                           ("bq", NHB * 128), ("bk", NHB * 128)):
                for c0 in range(0, sz, NG):
                    groups.append((OFF[n_] + c0, "qk", QO[n_] + c0, n_ == "mo"))
            for c0 in range(0, 3 * D, NG):
                groups.append((OFF["gates"] + c0, "gates", c0, True))

            def jobs(g):
                col0, dst, row0, sig = groups[g]

                def epi(tp0, g_, ct, accs, dst=dst, row0=row0, sig=sig):
                    for tt, tl in enumerate(accs[0]):
                        o = OST.next()
                        if sig:
                            p.op("act", lambda e, o=o, tl=tl: e.activation(out=o[:], in_=tl[:], func=AF.Sigmoid),
                                 reads=[tl.b], writes=[o.b])
                        else:
                            p.op("dve", lambda e, o=o, tl=tl: e.tensor_copy(out=o[:], in_=tl[:]), reads=[tl.b], writes=[o.b])
                        r0 = row0 + ct * 128
                        t0 = tp0 + tt * 512
                        p.dma("pool", sc[dst][r0:r0 + 128, t0:t0 + 512], o[:], o.b, reads=[o.b], writes=[dbuf(nm(dst))])
                return [(w_in[l], col0, 0)], epi
            TP = min(c.get("TPBIG", 2048), S)
            phase_gemm("zin", [(sc["hT"], nm("hT"))], D, TP, NG, jobs, len(groups), nws=2 if TP > 1024 else 4)
        if CUT <= 2:
            break

        with ExitStack() as st:
            NG = 256
            OST = Ring([cx.sb([128, NG], BF16, "zv_o", st) for _ in range(4)])
            OSF = Ring([cx.sb([128, 2 * NHM], F32, "zv_of", st) for _ in range(2)])
            groups = []
            for n_, sz in (("dv", NHD * 256), ("mv", NHM * 256), ("bv", NHB * 128)):
                for c0 in range(0, sz, NG):
                    groups.append((OFF[n_] + c0, VO[n_] + c0, NG))
            groups.append((OFF["mi"], -1, 2 * NHM))

            def jobs(g):
                col0, vcol, width = groups[g]

                def epi(tp0, g_, tt, tl, vcol=vcol, width=width):
                    t0 = tp0 + tt * 128
                    if vcol >= 0:
                        o = OST.next()
                        p.op("dve", lambda e, o=o, tl=tl: e.tensor_copy(out=o[:], in_=tl[:, 0:NG]), reads=[tl.b], writes=[o.b])
                        p.dma("pool", sc["v"][t0:t0 + 128, vcol:vcol + NG], o[:], o.b, reads=[o.b], writes=[dbuf(nm("v"))])
                    else:
                        o = OSF.next()
                        p.op("dve", lambda e, o=o, tl=tl: e.tensor_copy(out=o[:], in_=tl[:, 0:width]), reads=[tl.b], writes=[o.b])
                        p.dma("pool", sc["gif"][t0:t0 + 128, :], o[:], o.b, reads=[o.b], writes=[dbuf(nm("gif"))])
                return [(w_in[l], col0, 0)], epi
            TPv = min(c.get("TPBIG", 2048), S)
            phase_gemm("zv", [(sc["hT"], nm("hT"))], D, TPv, NG, jobs, len(groups), tokmajor=True, nws=2 if TPv > 1024 else 4)
        if CUT <= 3:
            break

        mixers(nc, p, cx, c, l, sc, nm, dbuf, dict(ones_bf=ones_bf, ones_f=ones_f, ident_f=ident_f, utri_f=utri_f, eps_t=eps_t),
               dict(diff_lambda=diff_lambda, diff_norm_g=diff_norm_g, ml_conv_w=ml_conv_w, ml_conv_b=ml_conv_b,
                    ml_gate_b=ml_gate_b, ml_norm_g=ml_norm_g), QO, VO)
        if CUT <= 6:
            break

        with ExitStack() as st:
            NG = 128
            GT = Ring([cx.sb([128, 512], BF16, "br_g", st) for _ in range(6)])
            MT = Ring([cx.sb([128, 512], F32, "br_m", st) for _ in range(2)])
            OT = Ring([cx.sb([128, 512], BF16, "br_o", st) for _ in range(2)])

            def jobs(g):
                col0 = g * NG

                def epi(tp0, g_, ct, accs, col0=col0):
                    r0 = col0 + ct * 128
                    for tt in range(len(accs[0])):
                        t0 = tp0 + tt * 512
                        gts = []
                        for b in range(3):
                            gt_ = GT.next()
                            p.dma("sp", gt_[:], sc["gates"][b * D + r0:b * D + r0 + 128, t0:t0 + 512], gt_.b,
                                  reads=[dbuf(nm("gates"))], writes=[gt_.b])
                            gts.append(gt_)
                        m = MT.next()
                        o = OT.next()
                        p.op("dve", lambda e, m=m, a=accs[0][tt], g0=gts[0]: e.tensor_tensor(out=m[:], in0=a[:], in1=g0[:], op=ALU.mult),
                             reads=[accs[0][tt].b, gts[0].b], writes=[m.b])
                        for b in (1, 2):
                            gt_ = gts[b]
                            p.op("dve", lambda e, a=accs[b][tt], gt_=gt_: e.tensor_tensor(out=gt_[:], in0=a[:], in1=gt_[:], op=ALU.mult),
                                 reads=[accs[b][tt].b, gt_.b], writes=[gt_.b])
                            p.op("pool", lambda e, m=m, gt_=gt_: e.tensor_tensor(out=m[:], in0=m[:], in1=gt_[:], op=ALU.add),
                                 reads=[m.b, gt_.b], writes=[m.b])
                        p.op("act", lambda e, o=o, m=m: e.copy(out=o[:], in_=m[:]), reads=[m.b], writes=[o.b])
                        p.dma("pool", sc["merged"][r0:r0 + 128, t0:t0 + 512], o[:], o.b, reads=[o.b], writes=[dbuf(nm("merged"))])
                return [(w_branch[l, b], col0, b) for b in range(3)], epi
            acts = [(sc["y"][b * BW:(b + 1) * BW, :], nm("y")) for b in range(3)]
            phase_gemm("br", acts, BW, min(1024, S), NG, jobs, D // NG)
        if CUT <= 7:
            break

        def resid_gemm(name, act_ap, act_name, K, TP, NG, w2d, xs, xs_name, xd, xd_name):
            with ExitStack() as st:
                XT = Ring([cx.sb([128, 512], F32, name + "_x", st) for _ in range(3)])

                def jobs(g):
                    col0 = g * NG

                    def epi(tp0, g_, ct, accs, col0=col0):
                        r0 = col0 + ct * 128
                        for tt, tl in enumerate(accs[0]):
                            t0 = tp0 + tt * 512
                            xt = XT.next()
                            p.dma("sp", xt[:], xs[r0:r0 + 128, t0:t0 + 512], xt.b, reads=[dbuf(xs_name)], writes=[xt.b])
                            p.op("dve", lambda e, xt=xt, tl=tl: e.tensor_tensor(out=xt[:], in0=tl[:], in1=xt[:], op=ALU.add),
                                 reads=[tl.b, xt.b], writes=[xt.b])
                            p.dma("pool", xd[r0:r0 + 128, t0:t0 + 512], xt[:], xt.b, reads=[xt.b], writes=[dbuf(xd_name)])
                    return [(w2d, col0, 0)], epi
                phase_gemm(name, [(act_ap, act_name)], K, TP, NG, jobs, D // NG, nws=2 if TP > 1024 else 4)

        resid_gemm("wo", sc["merged"], nm("merged"), D, min(c.get("TPBIG", 2048), S), 256, w_out[l], x_src, x_src_name, sc["x1"], nm("x1_"))

        phase_rmsnorm(sc["x1"], nm("x1_"), norm2_g[l:l + 1, :], sc["h2"], nm("h2_"))
        with ExitStack() as st:
            TPg = min(c.get("TPBIG", 2048), S)
            NG = 256 if (DFF % 256 == 0 and TPg <= 1024) else 128
            ST = Ring([cx.sb([128, 512], F32, "gu_s", st) for _ in range(2)])
            OT = Ring([cx.sb([128, 512], BF16, "gu_o", st) for _ in range(3)])

            def jobs(g):
                col0 = g * NG

                def epi(tp0, g_, ct, accs, col0=col0):
                    r0 = col0 + ct * 128
                    for tt in range(len(accs[0])):
                        t0 = tp0 + tt * 512
                        s_ = ST.next()
                        o = OT.next()
                        p.op("act", lambda e, s_=s_, a=accs[0][tt]: e.activation(out=s_[:], in_=a[:], func=AF.Silu),
                             reads=[accs[0][tt].b], writes=[s_.b])
                        p.op("dve", lambda e, o=o, s_=s_, a=accs[1][tt]: e.tensor_tensor(out=o[:], in0=a[:], in1=s_[:], op=ALU.mult),
                             reads=[accs[1][tt].b, s_.b], writes=[o.b])
                        p.dma("pool", sc["act"][r0:r0 + 128, t0:t0 + 512], o[:], o.b, reads=[o.b], writes=[dbuf(nm("act"))])
                return [(w_gate_up[l], col0, 0), (w_gate_up[l], DFF + col0, 0)], epi
            phase_gemm("gu", [(sc["h2"], nm("h2_"))], D, TPg, NG, jobs, DFF // NG)
        if DFF % 256 == 0 and (DFF // 2) % 128 == 0 and S >= 1024:
            K2 = DFF // 2
            resid_gemm("wd1", sc["act"][0:K2, :], nm("act"), K2, 1024, 256, w_down[l][0:K2, :], sc["x1"], nm("x1_"), sc["xh"], nm("xh"))
            resid_gemm("wd2", sc["act"][K2:DFF, :], nm("act"), K2, 1024, 256, w_down[l][K2:DFF, :], sc["xh"], nm("xh"), sc["x2"], nm("x2_"))
        else:
            resid_gemm("wd", sc["act"], nm("act"), DFF, 512, 128, w_down[l], sc["x1"], nm("x1_"), sc["x2"], nm("x2_"))

    if c.get("cut", 99) < 99:
        phase_rmsnorm(xT_in, "xT_in", final_g, yT_out, "yT_out", out_f32=True)
    else:
        phase_rmsnorm(scr[L - 1]["x2"], f"x2_{L - 1}", final_g, yT_out, "yT_out", out_f32=True)
    top.close()
    return nc


def mixers(nc, p, cx, c, l, sc, nm, dbuf, K, W, QO, VO):
    D, S, NHD, NHM, NHB, BLK, TOPK = c["D"], c["S"], c["NHD"], c["NHM"], c["NHB"], c["BLK"], c["TOPK"]
    BW = NHD * 256
    NT = S // 128
    NB = S // BLK
    SW = S + 384
    ones_bf, ones_f, ident_f, utri_f, eps_t = K["ones_bf"], K["ones_f"], K["ident_f"], K["utri_f"], K["eps_t"]
    scale = 128 ** -0.5
    lam_init = 0.8 - 0.6 * math.exp(-0.3 * l)

    def common(st, need_strip=True):
        T = {}
        T["B1"] = cx.sb([128, SW], F32, "B1", st)
        B1 = T["B1"]
        p.op("pool", lambda e: e.iota(B1[:], pattern=[[1, SW]], base=-384, channel_multiplier=-1,
                                      allow_small_or_imprecise_dtypes=True), writes=[B1.b])
        if need_strip:
            T["NEG"] = cx.sb([128, SW], F32, "NEG", st)
            NEG = T["NEG"]
            p.op("dve", lambda e: e.tensor_scalar(out=NEG[:], in0=B1[:], scalar1=0.0, scalar2=1000.0, op0=ALU.min, op1=ALU.mult),
                 reads=[B1.b], writes=[NEG.b])
            T["STRIP"] = cx.sb([128, SW], F32, "STRIP", st)
        T["qT"] = cx.sb([128, S], BF16, "qT", st)
        T["kT"] = cx.sb([128, S], BF16, "kT", st)
        T["V"] = cx.sb([128, NT, 256], BF16, "V", st)
        T["SPS"] = Ring([cx.ps([128, 512], F32, "sps", st) for _ in range(3)])
        T["ACC"] = [cx.ps([128, 512], F32, "acc", st) for _ in range(3)]
        T["AUX"] = cx.ps([128, 512], F32, "aux", st)
        T["AUX2"] = cx.ps([128, 512], F32, "aux2", st)
        T["TT"] = Ring([cx.sb([128, 512], F32, "T", st) for _ in range(3)])
        T["PT"] = Ring([cx.sb([128, 512], BF16, "PT", st) for _ in range(5)])
        T["RR"] = cx.sb([128, 512], F32, "RR", st)
        T["O0"] = [cx.sb([128, 512], F32, "O0", st) for _ in range(2)]
        T["YY"] = [cx.sb([128, 512], F32, "YY", st) for _ in range(2)]
        T["YO"] = Ring([cx.sb([128, 512], BF16, "YO", st) for _ in range(3)])
        return T

    def run_pipe(jobs, lag=2):
        n = len(jobs)
        res = [None] * n
        for i in range(n + lag):
            if i < n:
                res[i] = jobs[i][0]()
            if i >= lag:
                jobs[i - lag][1](res[i - lag])

    def load_head(T, q_row, k_row, v_col, dv):
        qT, kT, V = T["qT"], T["kT"], T["V"]
        if q_row is not None:
            p.dma("sp", qT[:], sc["qk"][q_row:q_row + 128, :], qT.b, reads=[dbuf(nm("qk"))], writes=[qT.b])
        if k_row is not None:
            p.dma("sp", kT[:], sc["qk"][k_row:k_row + 128, :], kT.b, reads=[dbuf(nm("qk"))], writes=[kT.b])
        if v_col is not None:
            vv = sc["v"][:, v_col:v_col + dv].rearrange("(n p) d -> p n d", p=128)
            for n0 in range(0, NT, 8):
                n1 = min(NT, n0 + 8)
                p.dma("sp", V[:, n0:n1, 0:dv], vv[:, n0:n1, :], V.b, reads=[dbuf(nm("v"))], writes=[V.b])

    def make_strip(T, slope):
        B1, NEG, STRIP = T["B1"], T["NEG"], T["STRIP"]
        p.op("dve", lambda e: e.scalar_tensor_tensor(out=STRIP[:], in0=B1[:], scalar=-slope, in1=NEG[:], op0=ALU.mult, op1=ALU.add),
             reads=[B1.b, NEG.b], writes=[STRIP.b])

    def pv_accumulate(T, pt, kt, ndv, first, last, QW):
        ACC, V = T["ACC"], T["V"]
        for j in range(ndv):
            p.op("pe", lambda e, j=j, pt=pt, kt=kt: e.matmul(ACC[j][:, 0:QW], V[:, kt, j * 128:(j + 1) * 128], pt[:, 0:QW], start=first, stop=last),
                 reads=[V.b, pt.b], writes=[ACC[j].b])
        p.op("pe", lambda e, pt=pt: e.matmul(ACC[2][:, 0:QW], ones_bf[:], pt[:, 0:QW], start=first, stop=last),
             reads=[ones_bf.b, pt.b], writes=[ACC[2].b])

    def attn_tile(T, q0, QW, kt, pre_mask=None):
        qT, kT, STRIP = T["qT"], T["kT"], T["STRIP"]
        sp = T["SPS"].next()
        k0 = kt * 128
        p.op("pe", lambda e, sp=sp: e.matmul(sp[:, 0:QW], kT[:, k0:k0 + 128], qT[:, q0:q0 + QW], start=True, stop=(pre_mask is None)),
             reads=[kT.b, qT.b], writes=[sp.b])
        if pre_mask is not None:
            esel, n, nst = pre_mask
            p.op("pe", lambda e, sp=sp: e.matmul(sp[:, 0:QW], esel[0:16, n * 128:(n + 1) * 128], nst[0:16, 0:QW], start=False, stop=True),
                 reads=[nst.b, esel.b], writes=[sp.b])
        t = T["TT"].next()
        off = q0 - k0 + 384
        p.op("dve", lambda e, sp=sp, t=t: e.scalar_tensor_tensor(out=t[:, 0:QW], in0=sp[:, 0:QW], scalar=scale,
                                                                 in1=STRIP[:, off:off + QW], op0=ALU.mult, op1=ALU.add),
             reads=[sp.b, STRIP.b], writes=[t.b])
        pt = T["PT"].next()
        p.op("act", lambda e, t=t, pt=pt: e.activation(out=pt[:, 0:QW], in_=t[:, 0:QW], func=AF.Exp), reads=[t.b], writes=[pt.b])
        return pt

    with ExitStack() as st:
        T = common(st)
        ACC, AUX, RR, O0, YY, YO = T["ACC"], T["AUX"], T["RR"], T["O0"], T["YY"], T["YO"]
        C0 = [cx.sb([128, S], F32, "C0", st) for _ in range(2)]
        SQ = [cx.sb([128, 512], BF16, "SQb", st) for _ in range(2)]
        lam_t = cx.sb([128, 4], F32, "lam", st)
        dl = cx.sb([128, 4], F32, "dl", st)
        p.dma("sp", dl[:], W["diff_lambda"][l].rearrange("f d -> d f"), dl.b, writes=[dl.b], slow=True)
        pr = cx.sb([128, 2], F32, "pr", st)
        p.op("dve", lambda e: e.tensor_tensor(out=pr[:, 0:1], in0=dl[:, 0:1], in1=dl[:, 1:2], op=ALU.mult), reads=[dl.b], writes=[pr.b])
        p.op("dve", lambda e: e.tensor_tensor(out=pr[:, 1:2], in0=dl[:, 2:3], in1=dl[:, 3:4], op=ALU.mult), reads=[dl.b, pr.b], writes=[pr.b])
        p.op("pe", lambda e: e.matmul(AUX[:, 0:2], ones_f[:], pr[:], start=True, stop=True), reads=[ones_f.b, pr.b], writes=[AUX.b])
        p.op("act", lambda e: e.activation(out=lam_t[:, 0:2], in_=AUX[:, 0:2], func=AF.Exp), reads=[AUX.b], writes=[lam_t.b])
        p.op("dve", lambda e: e.tensor_tensor(out=lam_t[:, 2:3], in0=lam_t[:, 1:2], in1=lam_t[:, 0:1], op=ALU.subtract),
             reads=[lam_t.b], writes=[lam_t.b])
        p.op("dve", lambda e: e.tensor_scalar(out=lam_t[:, 2:3], in0=lam_t[:, 2:3], scalar1=-lam_init, scalar2=None, op0=ALU.add),
             reads=[lam_t.b], writes=[lam_t.b])
        gd = cx.sb([128, 2], F32, "gd", st)
        p.dma("sp", gd[:], W["diff_norm_g"][l:l + 1, :].rearrange("o (j p) -> p (o j)", p=128), gd.b, writes=[gd.b], slow=True)
        p.op("dve", lambda e: e.tensor_scalar(out=gd[:], in0=gd[:], scalar1=(1.0 - lam_init), scalar2=None, op0=ALU.mult),
             reads=[gd.b], writes=[gd.b])
        def diff_epilogue(q0, comp, h):
            p.op("dve", lambda e: e.reciprocal(out=RR[:], in_=ACC[2][:]), reads=[ACC[2].b], writes=[RR.b])
            for j in range(2):
                if comp == 0:
                    p.op("dve", lambda e, j=j, q0=q0: e.tensor_tensor(out=C0[j][:, q0:q0 + 512], in0=ACC[j][:], in1=RR[:], op=ALU.mult),
                         reads=[ACC[j].b, RR.b], writes=[C0[j].b])
                else:
                    p.op("dve", lambda e, j=j: e.tensor_tensor(out=O0[j][:], in0=ACC[j][:], in1=RR[:], op=ALU.mult),
                         reads=[ACC[j].b, RR.b], writes=[O0[j].b])
                    p.op("dve", lambda e, j=j, q0=q0: e.scalar_tensor_tensor(out=YY[j][:], in0=O0[j][:], scalar=lam_t[:, 2:3],
                                                                             in1=C0[j][:, q0:q0 + 512], op0=ALU.mult, op1=ALU.add),
                         reads=[O0[j].b, lam_t.b, C0[j].b], writes=[YY[j].b])
                    p.op("act", lambda e, j=j: e.activation(out=SQ[j][:], in_=YY[j][:], func=AF.Square), reads=[YY[j].b], writes=[SQ[j].b])
            if comp == 1:
                for j in range(2):
                    p.op("pe", lambda e, j=j: e.matmul(AUX[:], ones_bf[:], SQ[j][:], start=(j == 0), stop=(j == 1)),
                         reads=[ones_bf.b, SQ[j].b], writes=[AUX.b])
                p.op("act", lambda e: e.activation(out=RR[:], in_=AUX[:], func=AF.Sqrt, bias=eps_t[:, 0:1], scale=1.0 / 256),
                     reads=[AUX.b, eps_t.b], writes=[RR.b])
                p.op("dve", lambda e: e.reciprocal(out=RR[:], in_=RR[:]), reads=[RR.b], writes=[RR.b])
                for j in range(2):
                    yo = YO.next()
                    p.op("dve", lambda e, j=j, yo=yo: e.scalar_tensor_tensor(out=yo[:], in0=YY[j][:], scalar=gd[:, j:j + 1], in1=RR[:],
                                                                             op0=ALU.mult, op1=ALU.mult),
                         reads=[YY[j].b, gd.b, RR.b], writes=[yo.b])
                    r0 = h * 256 + j * 128
                    p.dma("pool", sc["y"][r0:r0 + 128, q0:q0 + 512], yo[:], yo.b, reads=[yo.b], writes=[dbuf(nm("y"))])

        for h in range(NHD):
            make_strip(T, 2.0 ** (-8.0 * (h + 1) / NHD))
            for comp in range(2):
                load_head(T, QO["dq"] + h * 256 + comp * 128, QO["dk"] + h * 256 + comp * 128,
                          VO["dv"] + h * 256 if comp == 0 else None, 256)
                jobs = []
                for qi in range(S // 512):
                    q0 = qi * 512
                    nkt = (q0 + 512) // 128
                    for kt in range(nkt):
                        def s1(q0=q0, kt=kt):
                            return attn_tile(T, q0, 512, kt)

                        def s2(pt, q0=q0, kt=kt, nkt=nkt, comp=comp, h=h):
                            pv_accumulate(T, pt, kt, 2, kt == 0, kt == nkt - 1, 512)
                            if kt == nkt - 1:
                                diff_epilogue(q0, comp, h)
                        jobs.append((s1, s2))
                run_pipe(jobs)
        p.end_phase()

    if c.get("cut", 99) <= 4:
        return
    with ExitStack() as st:
        T = common(st)
        ACC, AUX, AUX2, RR, YO, qT, kT = T["ACC"], T["AUX"], T["AUX2"], T["RR"], T["YO"], T["qT"], T["kT"]
        NBP = max(NB, 8)
        ESEL = cx.sb([16, 16 * 128], BF16, "ESEL", st)
        ETMP = cx.sb([16, 16 * 128], F32, "ETMP", st)
        p.op("pool", lambda e: e.iota(ETMP[:].rearrange("p (n m) -> p n m", m=128), pattern=[[1, 16], [0, 128]], base=0, channel_multiplier=-1,
                                      allow_small_or_imprecise_dtypes=True), writes=[ETMP.b])
        p.op("dve", lambda e: e.tensor_single_scalar(out=ESEL[:], in_=ETMP[:], scalar=0.0, op=ALU.is_equal), reads=[ETMP.b], writes=[ESEL.b])
        KM = cx.sb([128, 16], F32, "KM", st)
        QF = cx.sb([128, BLK], F32, "QF", st)
        GS = cx.sb([128, 16], F32, "GS", st)
        MX = cx.sb([128, 8], F32, "MX", st)
        NS = cx.sb([128, 16], F32, "NS", st)
        NST = cx.sb([16, BLK], BF16, "NST", st)
        for h in range(NHB):
            make_strip(T, 2.0 ** (-8.0 * (h + 1) / NHB))
            load_head(T, QO["bq"] + h * 128, QO["bk"] + h * 128, VO["bv"] + h * 128, 128)
            p.op("pool", lambda e: e.memset(KM[:], 0.0), writes=[KM.b])
            p.op("dve", lambda e: e.tensor_reduce(out=KM[:, 0:NB], in_=kT[:].rearrange("p (n k) -> p n k", k=BLK), axis=mybir.AxisListType.X, op=ALU.add),
                 reads=[kT.b, KM.b], writes=[KM.b])
            p.op("dve", lambda e: e.tensor_scalar(out=KM[:], in0=KM[:], scalar1=1.0 / BLK, scalar2=None, op0=ALU.mult), reads=[KM.b], writes=[KM.b])
            jobs = []
            for j in range(NB):
                q0 = j * BLK
                masked = j > TOPK

                def prep(j=j, q0=q0):
                    p.op("dve", lambda e, q0=q0: e.tensor_copy(out=QF[:], in_=qT[:, q0:q0 + BLK]), reads=[qT.b], writes=[QF.b])
                    for hf in range(BLK // 128):
                        p.op("pe", lambda e, hf=hf: e.matmul(AUX[:, 0:16], QF[:, hf * 128:(hf + 1) * 128], KM[:, 0:16], start=True, stop=True),
                             reads=[QF.b, KM.b], writes=[AUX.b])
                        p.op("pool", lambda e: e.memset(GS[:], -1e30), writes=[GS.b])
                        p.op("dve", lambda e, j=j: e.tensor_copy(out=GS[:, 0:j], in_=AUX[:, 0:j]), reads=[AUX.b, GS.b], writes=[GS.b])
                        p.op("dve", lambda e: e.max(out=MX[:], in_=GS[:]), reads=[GS.b], writes=[MX.b])
                        p.op("dve", lambda e: e.tensor_scalar(out=NS[:], in0=GS[:], scalar1=MX[:, TOPK - 1:TOPK], scalar2=-30000.0,
                                                              op0=ALU.is_lt, op1=ALU.mult), reads=[GS.b, MX.b], writes=[NS.b])
                        p.op("pe", lambda e: e.transpose(AUX2[0:16, 0:128], NS[:], ident_f[:]), reads=[NS.b, ident_f.b], writes=[AUX2.b])
                        p.op("act", lambda e, hf=hf: e.copy(out=NST[:, hf * 128:(hf + 1) * 128], in_=AUX2[0:16, 0:128]), reads=[AUX2.b], writes=[NST.b])
                def epi(q0=q0, h=h):
                    p.op("dve", lambda e: e.reciprocal(out=RR[:, 0:BLK], in_=ACC[2][:, 0:BLK]), reads=[ACC[2].b], writes=[RR.b])
                    yo = YO.next()
                    p.op("dve", lambda e, yo=yo: e.tensor_tensor(out=yo[:, 0:BLK], in0=ACC[0][:, 0:BLK], in1=RR[:, 0:BLK], op=ALU.mult),
                         reads=[ACC[0].b, RR.b], writes=[yo.b])
                    r0 = 2 * BW + h * 128
                    p.dma("pool", sc["y"][r0:r0 + 128, q0:q0 + BLK], yo[:, 0:BLK], yo.b, reads=[yo.b], writes=[dbuf(nm("y"))])

                nkt = (q0 + BLK) // 128
                for kt in range(nkt):
                    n = (kt * 128) // BLK
                    pm = (ESEL, n, NST) if (masked and n < j) else None

                    def s1(q0=q0, kt=kt, pm=pm, prep=prep, masked=masked):
                        if kt == 0 and masked:
                            prep()
                        return attn_tile(T, q0, BLK, kt, pm)

                    def s2(pt, kt=kt, nkt=nkt, epi=epi):
                        pv_accumulate(T, pt, kt, 1, kt == 0, kt == nkt - 1, BLK)
                        if kt == nkt - 1:
                            epi()
                    jobs.append((s1, s2))
            run_pipe(jobs)
        p.end_phase()

    if c.get("cut", 99) <= 5:
        return
    with ExitStack() as st:
        T = common(st, need_strip=False)
        ACC, AUX, AUX2, RR, O0, YY, YO, qT, kT, B1 = T["ACC"], T["AUX"], T["AUX2"], T["RR"], T["O0"], T["YY"], T["YO"], T["qT"], T["kT"], T["B1"]
        CM = cx.sb([128, SW], BF16, "CM", st)
        p.op("dve", lambda e: e.tensor_single_scalar(out=CM[:], in_=B1[:], scalar=0.0, op=ALU.is_ge), reads=[B1.b], writes=[CM.b])
        H2 = 2 * NHM
        GIF = cx.sb([128, NT, H2], F32, "GIF", st)
        GB = cx.sb([128, NT, H2], F32, "GB", st)
        gv = sc["gif"].rearrange("(n p) c -> p n c", p=128)
        for n0 in range(0, NT, 8):
            n1 = min(NT, n0 + 8)
            p.dma("sp", GIF[:, n0:n1, :], gv[:, n0:n1, :], GIF.b, reads=[dbuf(nm("gif"))], writes=[GIF.b], slow=True)
        gb_src = bass.AP(tensor=W["ml_gate_b"].tensor, offset=W["ml_gate_b"][l].offset, ap=[[0, 128], [0, NT], [1, H2]])
        p.dma("sp", GB[:], gb_src, GB.b, writes=[GB.b], slow=True)
        p.op("dve", lambda e: e.tensor_tensor(out=GIF[:], in0=GIF[:], in1=GB[:], op=ALU.add), reads=[GIF.b, GB.b], writes=[GIF.b])
        LF = cx.sb([128, NT, NHM], F32, "LF", st)
        p.op("act", lambda e: e.activation(out=LF[:], in_=GIF[:, :, NHM:H2], func=AF.Exp, scale=-1.0), reads=[GIF.b], writes=[LF.b])
        p.op("act", lambda e: e.activation(out=LF[:], in_=LF[:], func=AF.Ln, bias=eps_t[:, 1:2], scale=1.0), reads=[LF.b, eps_t.b], writes=[LF.b])
        p.op("dve", lambda e: e.tensor_scalar(out=LF[:], in0=LF[:], scalar1=-1.0, scalar2=None, op0=ALU.mult), reads=[LF.b], writes=[LF.b])
        NC_ = NT * NHM
        G = cx.sb([128, NT, NHM], F32, "G", st)
        TOTS = cx.sb([128, NT, NHM], F32, "TOTS", st)
        PRE = cx.sb([128, NT, NHM], F32, "PRE", st)
        U = cx.sb([128, NT, NHM], F32, "U", st)
        for c0 in range(0, NC_, 512):
            c1 = min(NC_, c0 + 512)
            lf2 = LF[:].rearrange("p n h -> p (n h)")
            p.op("pe", lambda e, c0=c0, c1=c1, lf2=lf2: e.matmul(AUX[:, 0:c1 - c0], utri_f[:], lf2[:, c0:c1], start=True, stop=True),
                 reads=[utri_f.b, LF.b], writes=[AUX.b])
            p.op("pe", lambda e, c0=c0, c1=c1, lf2=lf2: e.matmul(AUX2[:, 0:c1 - c0], ones_f[:], lf2[:, c0:c1], start=True, stop=True),
                 reads=[ones_f.b, LF.b], writes=[AUX2.b])
            p.op("dve", lambda e, c0=c0, c1=c1: e.tensor_copy(out=G[:].rearrange("p n h -> p (n h)")[:, c0:c1], in_=AUX[:, 0:c1 - c0]),
                 reads=[AUX.b], writes=[G.b])
            p.op("dve", lambda e, c0=c0, c1=c1: e.tensor_copy(out=TOTS[:].rearrange("p n h -> p (n h)")[:, c0:c1], in_=AUX2[:, 0:c1 - c0]),
                 reads=[AUX2.b], writes=[TOTS.b])
        p.op("pool", lambda e: e.memset(PRE[:], 0.0), writes=[PRE.b])
        for n in range(1, NT):
            p.op("dve", lambda e, n=n: e.tensor_tensor(out=PRE[:, n, :], in0=PRE[:, n - 1, :], in1=TOTS[:, n - 1, :], op=ALU.add),
                 reads=[PRE.b, TOTS.b], writes=[PRE.b])
        p.op("dve", lambda e: e.tensor_tensor(out=G[:], in0=G[:], in1=PRE[:], op=ALU.add), reads=[G.b, PRE.b], writes=[G.b])
        p.op("dve", lambda e: e.tensor_tensor(out=U[:], in0=GIF[:, :, 0:NHM], in1=G[:], op=ALU.subtract), reads=[GIF.b, G.b], writes=[U.b])
        GBC = cx.sb([128, S], F32, "GBC", st)
        DG = Ring([cx.sb([128, 128], F32, "DG", st) for _ in range(2)])
        XP = cx.sb([128, S + 3], BF16, "XP", st)
        CA = cx.sb([128, S], F32, "CA", st)
        cw = cx.sb([128, 4], F32, "cw", st)
        cb = cx.sb([128, 1], F32, "cb", st)
        gm = cx.sb([128, 2], F32, "gm", st)
        DT = Ring([cx.sb([128, 512], F32, "DT", st) for _ in range(2)])
        OG = Ring([cx.sb([128, 512], BF16, "OG", st) for _ in range(2)])
        SQF = [cx.sb([128, 512], F32, "SQf", st) for _ in range(2)]
        MEAN = cx.sb([128, 512], F32, "MEAN", st)
        p.op("pool", lambda e: e.memset(XP[:, 0:3], 0.0), writes=[XP.b])

        def conv_silu(row, ch0, dst):
            p.dma("sp", XP[:, 3:S + 3], sc["qk"][row:row + 128, :], XP.b, reads=[dbuf(nm("qk"))], writes=[XP.b])
            p.dma("sp", cw[:], W["ml_conv_w"][l][:, ch0:ch0 + 128].rearrange("j c -> c j"), cw.b, writes=[cw.b], slow=True)
            p.dma("sp", cb[:], W["ml_conv_b"][l:l + 1, ch0:ch0 + 128].rearrange("o c -> c o"), cb.b, writes=[cb.b], slow=True)
            p.op("dve", lambda e: e.tensor_scalar(out=CA[:], in0=XP[:, 0:S], scalar1=cw[:, 0:1], scalar2=None, op0=ALU.mult),
                 reads=[XP.b, cw.b], writes=[CA.b])
            for jj in range(1, 4):
                p.op("dve", lambda e, jj=jj: e.scalar_tensor_tensor(out=CA[:], in0=XP[:, jj:jj + S], scalar=cw[:, jj:jj + 1], in1=CA[:],
                                                                    op0=ALU.mult, op1=ALU.add), reads=[XP.b, cw.b, CA.b], writes=[CA.b])
            p.op("act", lambda e: e.activation(out=dst[:], in_=CA[:], func=AF.Silu, bias=cb[:, 0:1], scale=1.0), reads=[CA.b, cb.b], writes=[dst.b])

        def ml_tile(q0, kt, h):
            k0 = kt * 128
            sp = T["SPS"].next()
            p.op("pe", lambda e, sp=sp, k0=k0, q0=q0: e.matmul(sp[:], kT[:, k0:k0 + 128], qT[:, q0:q0 + 512], start=True, stop=True),
                 reads=[kT.b, qT.b], writes=[sp.b])
            dt = DT.next()
            p.op("act", lambda e, dt=dt, q0=q0, kt=kt, h=h: e.activation(out=dt[:], in_=GBC[:, q0:q0 + 512], func=AF.Exp,
                                                                         bias=U[:, kt, h:h + 1], scale=1.0), reads=[GBC.b, U.b], writes=[dt.b])
            pt = T["PT"].next()
            if k0 + 128 > q0:
                off = q0 - k0 + 384
                p.op("pool", lambda e, dt=dt, off=off: e.tensor_tensor(out=dt[:], in0=dt[:], in1=CM[:, off:off + 512], op=ALU.mult),
                     reads=[dt.b, CM.b], writes=[dt.b])
            p.op("dve", lambda e, sp=sp, dt=dt, pt=pt: e.scalar_tensor_tensor(out=pt[:], in0=sp[:], scalar=scale, in1=dt[:], op0=ALU.mult, op1=ALU.mult),
                 reads=[sp.b, dt.b], writes=[pt.b])
            return pt

        def ml_epilogue(q0, h):
            p.op("act", lambda e: e.activation(out=RR[:], in_=ACC[2][:], func=AF.Abs), reads=[ACC[2].b], writes=[RR.b])
            p.op("dve", lambda e: e.tensor_scalar_max(out=RR[:], in0=RR[:], scalar1=1.0), reads=[RR.b], writes=[RR.b])
            p.op("dve", lambda e: e.reciprocal(out=RR[:], in_=RR[:]), reads=[RR.b], writes=[RR.b])
            for j in range(2):
                p.op("dve", lambda e, j=j: e.tensor_tensor(out=O0[j][:], in0=ACC[j][:], in1=RR[:], op=ALU.mult),
                     reads=[ACC[j].b, RR.b], writes=[O0[j].b])
                p.op("act", lambda e, j=j: e.activation(out=SQF[j][:], in_=O0[j][:], func=AF.Square), reads=[O0[j].b], writes=[SQF[j].b])
            for j in range(2):
                p.op("pe", lambda e, j=j: e.matmul(AUX[:], ones_f[:], O0[j][:], start=(j == 0), stop=(j == 1)),
                     reads=[ones_f.b, O0[j].b], writes=[AUX.b])
            for j in range(2):
                p.op("pe", lambda e, j=j: e.matmul(AUX2[:], ones_f[:], SQF[j][:], start=(j == 0), stop=(j == 1)),
                     reads=[ones_f.b, SQF[j].b], writes=[AUX2.b])
            p.op("dve", lambda e: e.tensor_scalar(out=MEAN[:], in0=AUX[:], scalar1=1.0 / 256, scalar2=None, op0=ALU.mult), reads=[AUX.b], writes=[MEAN.b])
            p.op("dve", lambda e: e.tensor_tensor(out=RR[:], in0=MEAN[:], in1=MEAN[:], op=ALU.mult), reads=[MEAN.b], writes=[RR.b])
            p.op("dve", lambda e: e.scalar_tensor_tensor(out=RR[:], in0=AUX2[:], scalar=1.0 / 256, in1=RR[:], op0=ALU.mult, op1=ALU.subtract),
                 reads=[AUX2.b, RR.b], writes=[RR.b])
            p.op("act", lambda e: e.activation(out=RR[:], in_=RR[:], func=AF.Sqrt, bias=eps_t[:, 0:1], scale=1.0), reads=[RR.b, eps_t.b], writes=[RR.b])
            p.op("dve", lambda e: e.reciprocal(out=RR[:], in_=RR[:]), reads=[RR.b], writes=[RR.b])
            for j in range(2):
                og = OG.next()
                r0 = QO["mo"] + h * 256 + j * 128
                p.dma("sp", og[:], sc["qk"][r0:r0 + 128, q0:q0 + 512], og.b, reads=[dbuf(nm("qk"))], writes=[og.b])
                p.op("dve", lambda e, j=j: e.tensor_tensor(out=YY[j][:], in0=O0[j][:], in1=MEAN[:], op=ALU.subtract), reads=[O0[j].b, MEAN.b], writes=[YY[j].b])
                p.op("dve", lambda e, j=j: e.scalar_tensor_tensor(out=YY[j][:], in0=YY[j][:], scalar=gm[:, j:j + 1], in1=RR[:], op0=ALU.mult, op1=ALU.mult),
                     reads=[YY[j].b, gm.b, RR.b], writes=[YY[j].b])
                yo = YO.next()
                p.op("dve", lambda e, j=j, yo=yo, og=og: e.tensor_tensor(out=yo[:], in0=YY[j][:], in1=og[:], op=ALU.mult), reads=[YY[j].b, og.b], writes=[yo.b])
                r1 = BW + h * 256 + j * 128
                p.dma("pool", sc["y"][r1:r1 + 128, q0:q0 + 512], yo[:], yo.b, reads=[yo.b], writes=[dbuf(nm("y"))])

        for h in range(NHM):
            conv_silu(QO["mq"] + h * 128, h * 128, qT)
            conv_silu(QO["mk"] + h * 128, NHM * 128 + h * 128, kT)
            load_head(T, None, None, VO["mv"] + h * 256, 256)
            p.dma("sp", gm[:], W["ml_norm_g"][l:l + 1, h * 256:(h + 1) * 256].rearrange("o (j p) -> p (o j)", p=128), gm.b, writes=[gm.b], slow=True)
            for n in range(NT):
                dg = DG.next()
                p.op("dve", lambda e, dg=dg, n=n, h=h: e.tensor_scalar(out=dg[:], in0=ident_f[:], scalar1=G[:, n, h:h + 1], scalar2=None, op0=ALU.mult),
                     reads=[ident_f.b, G.b], writes=[dg.b])
                p.op("pe", lambda e, dg=dg, n=n: e.matmul(AUX[:, (n % 4) * 128:(n % 4 + 1) * 128], ones_f[:], dg[:], start=True, stop=True),
                     reads=[ones_f.b, dg.b], writes=[AUX.b])
                if n % 4 == 3 or n == NT - 1:
                    n0 = (n // 4) * 4
                    w = (n - n0 + 1) * 128
                    p.op("act", lambda e, n0=n0, w=w: e.copy(out=GBC[:, n0 * 128:n0 * 128 + w], in_=AUX[:, 0:w]), reads=[AUX.b], writes=[GBC.b])
            jobs = []
            for qi in range(S // 512):
                q0 = qi * 512
                nkt = (q0 + 512) // 128
                for kt in range(nkt):
                    def s1(q0=q0, kt=kt, h=h):
                        return ml_tile(q0, kt, h)

                    def s2(pt, q0=q0, kt=kt, nkt=nkt, h=h):
                        pv_accumulate(T, pt, kt, 2, kt == 0, kt == nkt - 1, 512)
                        if kt == nkt - 1:
                            ml_epilogue(q0, h)
                    jobs.append((s1, s2))
            run_pipe(jobs)
        p.end_phase()


_CACHE = {}


REAL_CORES = (0, 1, 4, 5)


def kernel(**inputs):
    c = cfg_full()
    return run_cfg(c, inputs, n_cores=4, spread=True)


def run_cfg(c, inputs, n_cores, spread=False):
    key = tuple(sorted(c.items()))
    if key not in _CACHE:
        _CACHE[key] = build(c)
    nc = _CACHE[key]
    x = np.asarray(inputs["x"])
    B = x.shape[0]
    assert B == n_cores
    shared = {k: np.ascontiguousarray(np.asarray(v)) for k, v in inputs.items() if k != "x"}
    shared["final_g"] = shared["final_g"].reshape(1, -1)
    in_maps = []
    for b in range(B):
        m = dict(shared)
        m["xT"] = np.ascontiguousarray(x[b].T)
        in_maps.append(m)
    slots = list(range(B))
    if spread:
        zero = {k: np.zeros_like(v) for k, v in in_maps[0].items()}
        full = [zero] * 8
        full = list(full)
        for b, cidx in enumerate(REAL_CORES):
            full[cidx] = in_maps[b]
        in_maps = full
        slots = list(REAL_CORES)
    res = run_bass_kernel_spmd(nc, in_maps, core_ids=list(range(len(in_maps))))
    out = np.stack([np.ascontiguousarray(res.results[slots[b]]["yT"].T) for b in range(B)], axis=0)
    return out.astype(np.float32)
```
